# Optimizing a Trainium2 kernel written in Bass

```python
import jax, jax.numpy as jnp
from jax import lax
import numpy as np

D_MODEL = 1024
BATCH = 4
SEQ = 4096
DEPTH = 1

MEM_LEN = 256
EPS = 1e-6
CONV_DIM = 512
CONV_WIDTH = 31
FOX_HEADS = 8
FOX_HEAD_DIM = 64
FOX_DIM = FOX_HEADS * FOX_HEAD_DIM
Q_BLOCK = 128
XA_HEADS = 4
XA_HEAD_DIM = 128
XA_DIM = XA_HEADS * XA_HEAD_DIM
N_GROUPS = 4
EXPERTS_PER_GROUP = 4
N_EXPERTS = N_GROUPS * EXPERTS_PER_GROUP
TOP_K = 2
EXPERT_FF = 256
SPLITS = (2 * CONV_DIM, FOX_DIM, FOX_DIM, FOX_DIM, FOX_HEADS, D_MODEL, D_MODEL)
IN_COLS = sum(SPLITS)

kernel_name = "hybrid_conv_fox_xattn_hmoe"


def rmsnorm(x, g):
    xf = x.astype(jnp.float32)
    y = xf * lax.rsqrt(jnp.mean(xf * xf, axis=-1, keepdims=True) + EPS)
    return (y * g.astype(jnp.float32)).astype(x.dtype)


def layernorm(x, g, b):
    xf = x.astype(jnp.float32)
    mu = jnp.mean(xf, axis=-1, keepdims=True)
    var = jnp.mean(jnp.square(xf - mu), axis=-1, keepdims=True)
    y = (xf - mu) * lax.rsqrt(var + EPS)
    return (y * g.astype(jnp.float32) + b.astype(jnp.float32)).astype(x.dtype)


def conformer_conv(u_glu, dw_w, dw_b, ln_g, ln_b, pw_w):
    a, gate = jnp.split(u_glu, 2, axis=-1)
    u = a * jax.nn.sigmoid(gate)
    u = lax.conv_general_dilated(
        u, dw_w[:, None, :].astype(u.dtype), window_strides=(1,),
        padding=[(CONV_WIDTH - 1, 0)],
        dimension_numbers=("NWC", "WIO", "NWC"),
        feature_group_count=CONV_DIM) + dw_b
    u = jax.nn.silu(layernorm(u, ln_g, ln_b))
    return u @ pw_w


def fox_attention(q, k, v, logf):
    B, T, H, dh = q.shape
    nb = T // Q_BLOCK
    scale = dh ** -0.5
    c = jnp.cumsum(logf, axis=1).transpose(0, 2, 1)
    qh = q.transpose(0, 2, 1, 3)
    kh = k.transpose(0, 2, 1, 3)
    vh = v.transpose(0, 2, 1, 3)
    qb = qh.reshape(B, H, nb, Q_BLOCK, dh).transpose(2, 0, 1, 3, 4)
    cb = c.reshape(B, H, nb, Q_BLOCK).transpose(2, 0, 1, 3)
    kpos = jnp.arange(T)

    def block(args):
        q_i, c_i, i = args
        s = jnp.einsum("bhqd,bhkd->bhqk", q_i, kh).astype(jnp.float32) * scale
        s = s + c_i[..., :, None] - c[:, :, None, :]
        qpos = i * Q_BLOCK + jnp.arange(Q_BLOCK)
        s = jnp.where(kpos[None, :] <= qpos[:, None], s, -jnp.inf)
        p = jax.nn.softmax(s, axis=-1)
        return jnp.einsum("bhqk,bhkd->bhqd", p.astype(vh.dtype), vh)

    o = lax.map(block, (qb, cb, jnp.arange(nb)))
    return o.transpose(1, 0, 3, 2, 4).reshape(B, T, H * dh)


def cross_attention(h, m, wq, wk, wv, wo):
    B, T, _ = h.shape
    M = m.shape[1]
    q = (h @ wq).reshape(B, T, XA_HEADS, XA_HEAD_DIM)
    k = (m @ wk).reshape(B, M, XA_HEADS, XA_HEAD_DIM)
    v = (m @ wv).reshape(B, M, XA_HEADS, XA_HEAD_DIM)
    s = jnp.einsum("bthd,bmhd->bhtm", q, k).astype(jnp.float32) * XA_HEAD_DIM ** -0.5
    p = jax.nn.softmax(s, axis=-1)
    o = jnp.einsum("bhtm,bmhd->bthd", p.astype(v.dtype), v).reshape(B, T, XA_DIM)
    return o @ wo


def hierarchical_moe(h, wg, bg, we, be, w_gate, w_up, w_down):
    B, T, D = h.shape
    hf = h.reshape(B * T, D)
    g_logits = (hf @ wg).astype(jnp.float32) + bg.astype(jnp.float32)
    p_group = jax.nn.softmax(g_logits, axis=-1)
    g_star = jnp.argmax(p_group, axis=-1)
    p_gsel = jnp.take_along_axis(p_group, g_star[:, None], axis=-1)[:, 0]
    e_logits = ((hf @ we).astype(jnp.float32) + be.astype(jnp.float32)).reshape(-1, N_GROUPS, EXPERTS_PER_GROUP)
    e_sel = jnp.take_along_axis(e_logits, g_star[:, None, None], axis=1)[:, 0]
    p_exp = jax.nn.softmax(e_sel, axis=-1)
    top_v, top_i = lax.top_k(p_exp, TOP_K)
    top_v = top_v / jnp.sum(top_v, axis=-1, keepdims=True)
    weights = p_gsel[:, None] * top_v
    expert_idx = g_star[:, None] * EXPERTS_PER_GROUP + top_i
    combine = jnp.sum(jax.nn.one_hot(expert_idx, N_EXPERTS, dtype=jnp.float32) * weights[..., None], axis=1)
    a = jnp.einsum("nd,edf->nef", hf, w_gate)
    b = jnp.einsum("nd,edf->nef", hf, w_up)
    u = jax.nn.silu(a) * b * combine.astype(hf.dtype)[..., None]
    y = jnp.einsum("nef,efd->nd", u, w_down)
    return y.reshape(B, T, D)


def setup_inputs(seed: int = 0) -> dict:
    key = jax.random.key(seed)
    ks = jax.random.split(key, 32)
    f32 = jnp.float32

    def nrm(k, shape, fan_in):
        return jax.random.normal(k, shape, f32) * (fan_in ** -0.5)

    def gain(k, shape):
        return 1.0 + 0.1 * jax.random.normal(k, shape, f32)

    def small(k, shape, s=0.01):
        return s * jax.random.normal(k, shape, f32)

    L = DEPTH
    return {
        "x": jax.random.normal(ks[0], (BATCH, SEQ, D_MODEL), f32),
        "mem": jax.random.normal(ks[1], (BATCH, MEM_LEN, D_MODEL), f32),
        "norm_mix_g": gain(ks[2], (L, D_MODEL)),
        "w_in": nrm(ks[3], (L, D_MODEL, IN_COLS), D_MODEL),
        "fox_bf": jax.random.uniform(ks[4], (L, FOX_HEADS), f32, 1.0, 4.0),
        "conv_dw_w": nrm(ks[5], (L, CONV_WIDTH, CONV_DIM), CONV_WIDTH),
        "conv_dw_b": small(ks[6], (L, CONV_DIM)),
        "conv_ln_g": gain(ks[7], (L, CONV_DIM)),
        "conv_ln_b": small(ks[8], (L, CONV_DIM)),
        "conv_pw_w": nrm(ks[9], (L, CONV_DIM, D_MODEL), CONV_DIM),
        "attn_branch_w": nrm(ks[10], (L, FOX_DIM, D_MODEL), FOX_DIM),
        "w_out": nrm(ks[11], (L, D_MODEL, D_MODEL), D_MODEL),
        "norm_xa_g": gain(ks[12], (L, D_MODEL)),
        "norm_mem_g": gain(ks[13], (L, D_MODEL)),
        "xa_wq": nrm(ks[14], (L, D_MODEL, XA_DIM), D_MODEL),
        "xa_wk": nrm(ks[15], (L, D_MODEL, XA_DIM), D_MODEL),
        "xa_wv": nrm(ks[16], (L, D_MODEL, XA_DIM), D_MODEL),
        "xa_wo": nrm(ks[17], (L, XA_DIM, D_MODEL), XA_DIM),
        "norm_moe_g": gain(ks[18], (L, D_MODEL)),
        "router_group_w": nrm(ks[19], (L, D_MODEL, N_GROUPS), D_MODEL),
        "router_group_b": small(ks[20], (L, N_GROUPS)),
        "router_expert_w": nrm(ks[21], (L, D_MODEL, N_EXPERTS), D_MODEL),
        "router_expert_b": small(ks[22], (L, N_EXPERTS)),
        "expert_w_gate": nrm(ks[23], (L, N_EXPERTS, D_MODEL, EXPERT_FF), D_MODEL),
        "expert_w_up": nrm(ks[24], (L, N_EXPERTS, D_MODEL, EXPERT_FF), D_MODEL),
        "expert_w_down": nrm(ks[25], (L, N_EXPERTS, EXPERT_FF, D_MODEL), EXPERT_FF),
        "norm_final_g": gain(ks[26], (D_MODEL,)),
    }


def reference(x, mem, norm_mix_g, w_in, fox_bf, conv_dw_w, conv_dw_b, conv_ln_g, conv_ln_b,
              conv_pw_w, attn_branch_w, w_out, norm_xa_g, norm_mem_g, xa_wq, xa_wk, xa_wv, xa_wo,
              norm_moe_g, router_group_w, router_group_b, router_expert_w, router_expert_b,
              expert_w_gate, expert_w_up, expert_w_down, norm_final_g):
    B, T, _ = x.shape
    cuts = np.cumsum(SPLITS)[:-1].tolist()
    for l in range(DEPTH):
        h = rmsnorm(x, norm_mix_g[l])
        proj = h @ w_in[l]
        u_glu, q, k, v, f_logit, gate_c, gate_a = jnp.split(proj, cuts, axis=-1)
        conv_out = conformer_conv(u_glu, conv_dw_w[l], conv_dw_b[l], conv_ln_g[l],
                                  conv_ln_b[l], conv_pw_w[l])
        logf = jax.nn.log_sigmoid(f_logit.astype(jnp.float32) + fox_bf[l].astype(jnp.float32))
        attn = fox_attention(q.reshape(B, T, FOX_HEADS, FOX_HEAD_DIM),
                             k.reshape(B, T, FOX_HEADS, FOX_HEAD_DIM),
                             v.reshape(B, T, FOX_HEADS, FOX_HEAD_DIM), logf)
        attn_out = attn @ attn_branch_w[l]
        merged = jax.nn.sigmoid(gate_c) * conv_out + jax.nn.sigmoid(gate_a) * attn_out
        x = x + merged @ w_out[l]
        x = x + cross_attention(rmsnorm(x, norm_xa_g[l]), rmsnorm(mem, norm_mem_g[l]),
                                xa_wq[l], xa_wk[l], xa_wv[l], xa_wo[l])
        x = x + hierarchical_moe(rmsnorm(x, norm_moe_g[l]), router_group_w[l], router_group_b[l],
                                 router_expert_w[l], router_expert_b[l], expert_w_gate[l],
                                 expert_w_up[l], expert_w_down[l])
    return rmsnorm(x, norm_final_g)
```

```python
import contextlib
import numpy as np
import concourse.bass as bass
import concourse.mybir as mybir
from concourse.bass_utils import run_bass_kernel_spmd

F32 = mybir.dt.float32
BF16 = mybir.dt.bfloat16
AF = mybir.ActivationFunctionType
ALU = mybir.AluOpType

ENGS = ("pe", "act", "dve", "pool", "sp")
EPS = 1e-6
D = 1024
TOWN = 2048
NCORES = 8
EMBED_WAIT = True


class Prog:
    def __init__(self, nc):
        self.nc = nc
        self.outer = contextlib.ExitStack()
        self.stacks = [self.outer]
        self.ops = {e: [] for e in ENGS}
        self.cnt = {e: 0 for e in ENGS}
        self.esem = {e: self.outer.enter_context(nc.semaphore("s_" + e)) for e in ENGS}
        self.dsem = {}
        self.dcnt = {}
        self.last_w = {}
        self.readers = {}
        self.waited = {e: {} for e in ENGS}
        self.bank_rr = {}

    def push(self):
        self.stacks.append(contextlib.ExitStack())

    def pop(self):
        self.stacks.pop().close()

    def sb(self, name, shape, dt, outer=False):
        st = self.outer if outer else self.stacks[-1]
        return st.enter_context(self.nc.sbuf_tensor("sb_" + name, list(shape), dt))

    def ps(self, name, shape, dt):
        return self.outer.enter_context(self.nc.psum_tensor(name, list(shape), dt))

    def _dsem(self, name):
        if name not in self.dsem:
            self.dsem[name] = self.outer.enter_context(self.nc.semaphore("d_" + name))
            self.dcnt[name] = 0
        return self.dsem[name]

    def _deps(self, eng, reads, writes):
        toks = []
        for k in reads:
            t = self.last_w.get(k)
            if t is not None:
                toks.append(t)
        for k in writes:
            t = self.last_w.get(k)
            if t is not None:
                toks.append(t)
            toks.extend(self.readers.get(k, ()))
        need = {}
        for (sem, val, owner) in toks:
            if owner == eng and eng in ("pe", "sp"):
                continue
            sid = id(sem)
            if self.waited[eng].get(sid, 0) >= val:
                continue
            if sid not in need or need[sid][1] < val:
                need[sid] = (sem, val)
        for sid, (sem, val) in need.items():
            self.waited[eng][sid] = val
        return list(need.values())

    def _commit(self, tok, reads, writes):
        for k in reads:
            self.readers.setdefault(k, []).append(tok)
        for k in writes:
            self.last_w[k] = tok
            self.readers[k] = []

    def op(self, eng, fn, reads=(), writes=()):
        waits = self._deps(eng, reads, writes)
        self.cnt[eng] += 1
        val = self.cnt[eng]
        sem = self.esem[eng]

        def emit(h, fn=fn, waits=waits, sem=sem):
            if EMBED_WAIT and waits:
                for (s, v) in waits[1:]:
                    h.wait_ge(s, v)
                ins = fn(h)
                ins._wait_ge(waits[0][0], waits[0][1])
                ins.then_inc(sem, 1)
                return
            for (s, v) in waits:
                h.wait_ge(s, v)
            fn(h).then_inc(sem, 1)

        self.ops[eng].append(emit)
        self._commit((sem, val, eng), reads, writes)

    def dma(self, q, out, in_, sem_name, reads=(), writes=()):
        waits = self._deps(q, reads, writes)
        sem = self._dsem(sem_name)
        self.dcnt[sem_name] += 16
        val = self.dcnt[sem_name]

        def emit(h, waits=waits, sem=sem, out=out, in_=in_):
            for (s, v) in waits:
                h.wait_ge(s, v)
            h.dma_start(out=out, in_=in_).then_inc(sem, 16)

        self.ops[q].append(emit)
        self._commit((sem, val, "dma:" + sem_name), reads, writes)

    def collective(self, kind, ins, outs, groups, reads, writes):
        waits = self._deps("pool", reads, writes)
        sem = self._dsem("cc")
        self.dcnt["cc"] += 1
        val = self.dcnt["cc"]

        def emit(h, waits=waits, sem=sem):
            for (s, v) in waits:
                h.wait_ge(s, v)
            h.collective_compute(kind, ALU.bypass, replica_groups=groups, ins=ins, outs=outs).then_inc(sem, 1)

        self.ops["pool"].append(emit)
        self._commit((sem, val, "dma:cc"), reads, writes)

    def barrier(self):
        allw = [(self.esem[e], self.cnt[e], e) for e in ENGS if self.cnt[e] > 0]
        allw += [(self.dsem[n], self.dcnt[n], "dma:" + n) for n in self.dsem if self.dcnt[n] > 0]
        for eng in ENGS:
            waits = []
            for (sem, val, owner) in allw:
                if owner == eng:
                    continue
                if self.waited[eng].get(id(sem), 0) >= val:
                    continue
                self.waited[eng][id(sem)] = val
                waits.append((sem, val))

            def emit(h, waits=waits):
                for (s, v) in waits:
                    h.wait_ge(s, v)

            self.ops[eng].append(emit)
        self.last_w = {}
        self.readers = {}

    def phase_begin(self):
        self.push()

    def phase_end(self):
        self.barrier()
        nc = self.nc
        ops = self.ops
        with nc.Block() as block:
            @block.tensor
            def _(h):
                for f in ops["pe"]:
                    f(h)

            @block.scalar
            def _(h):
                for f in ops["act"]:
                    f(h)

            @block.vector
            def _(h):
                for f in ops["dve"]:
                    f(h)

            @block.gpsimd
            def _(h):
                for f in ops["pool"]:
                    f(h)

            @block.sync
            def _(h):
                for f in ops["sp"]:
                    f(h)
        self.ops = {e: [] for e in ENGS}
        self.pop()

    def bank(self, group, ids):
        i = self.bank_rr.get(group, 0)
        self.bank_rr[group] = i + 1
        b = ids[i % len(ids)]
        return self.banks[b], ("bank", b)


def mm(P, out, lhsT, rhs, start, stop, reads, writes):
    P.op("pe", lambda h: h.matmul(out, lhsT, rhs, start=start, stop=stop), reads, writes)


def tr(P, out, in_, ident, reads, writes):
    P.op("pe", lambda h: h.transpose(out, in_, ident), reads, writes)


def act(P, out, in_, func, reads, writes, bias=None, scale=None, accum_out=None):
    kw = {}
    if bias is not None:
        kw["bias"] = bias
    if scale is not None:
        kw["scale"] = scale
    if accum_out is not None:
        kw["accum_out"] = accum_out
    P.op("act", lambda h: h.activation(out, in_, func, **kw), reads, writes)


def tt(P, eng, out, in0, in1, op, reads, writes):
    P.op(eng, lambda h: h.tensor_tensor(out, in0, in1, op), reads, writes)


def ts(P, eng, out, in0, s1, s2, op0, op1, reads, writes):
    if s2 is None:
        P.op(eng, lambda h: h.tensor_scalar(out, in0, s1, None, op0), reads, writes)
    else:
        P.op(eng, lambda h: h.tensor_scalar(out, in0, s1, s2, op0, op1), reads, writes)


def stt(P, eng, out, in0, scalar, in1, op0, op1, reads, writes):
    P.op(eng, lambda h: h.scalar_tensor_tensor(out, in0, scalar, in1, op0, op1), reads, writes)


def cp(P, eng, out, in_, reads, writes):
    if eng == "act":
        P.op("act", lambda h: h.copy(out, in_), reads, writes)
    else:
        P.op(eng, lambda h: h.tensor_copy(out, in_), reads, writes)


def memset(P, eng, ap, val, writes):
    P.op(eng, lambda h: h.memset(ap, val), (), writes)


def build_program():
    nc = bass.Bass("TRN2", target_bir_lowering=False)

    def din(name, shape):
        return nc.dram_tensor(name, list(shape), F32, kind="ExternalInput").ap()

    x_own = din("x_own", [TOWN, D])
    x_all = din("x_all", [2 * TOWN, D])
    x_halo = din("x_halo", [128, D])
    w_own = din("w_own", [D, 772])
    mem = din("mem", [256, D])
    flag_d = din("flag", [128, 1])
    w_in = din("w_in", [D, 4616])
    pw_w = din("conv_pw_w", [512, D])
    ab_w = din("attn_branch_w", [512, D])
    wout_w = din("w_out", [D, D])
    xq_w = din("xa_wq", [D, 512])
    xk_w = din("xa_wk", [D, 512])
    xv_w = din("xa_wv", [D, 512])
    xo_w = din("xa_wo", [512, D])
    eg_w = din("expert_w_gate", [16, D, 256])
    eu_w = din("expert_w_up", [16, D, 256])
    ed_w = din("expert_w_down", [16, 256, D])
    wr_d = din("wr", [D, 20])
    rb_d = din("rb_b", [128, 20])
    g_mix_d = din("g_mix_b", [128, D])
    g_xa_d = din("g_xa_b", [128, D])
    g_mem_d = din("g_mem_b", [128, D])
    g_moe_d = din("g_moe_b", [128, D])
    g_fin_d = din("g_fin_b", [128, D])
    bf_d = din("bf_b", [128, 4])
    dww_d = din("dw_w_t", [128, 4 * 31])
    dwb_d = din("dw_b_t", [128, 4])
    lng_d = din("ln_g_t", [128, 4])
    lnb_d = din("ln_b_t", [128, 4])
    ident_d = din("ident", [128, 128])
    uneg_d = din("uneg", [128, 128])
    e127_d = din("e127", [128, 128])
    mask_d = din("mask01", [128, 128])
    out_d = nc.dram_tensor("out", [TOWN, D], F32, kind="ExternalOutput").ap()
    ag_in = nc.dram_tensor("ag_in", [256, 2 * TOWN], BF16).ap()
    ag_out = nc.dram_tensor("ag_out", [512, 2 * TOWN], BF16).ap()
    if DEBUG:
        x1_d = nc.dram_tensor("x1_scr", [TOWN, D], F32, kind="ExternalOutput").ap()
    else:
        x1_d = nc.dram_tensor("x1_scr", [TOWN, D], F32).ap()

    dbg_outs = {}

    def dump(name, ap, shape):
        if not DEBUG:
            return
        t = nc.dram_tensor("dbg_" + name, list(shape), F32, kind="ExternalOutput").ap()
        dbg_outs[name] = t
        P.dma("pool", t, ap, "dbg_" + name)

    P = Prog(nc)
    P.banks = [P.ps("bk%d" % i, [128, 512], F32) for i in range(8)]
    ALLB = list(range(8))

    attnT = P.sb("attnT", [128, 4, TOWN], BF16, outer=True)
    idb = P.sb("idb", [128, 128], BF16, outer=True)
    ones_bf = P.sb("ones_bf", [128, 128], BF16, outer=True)
    ones_f = P.sb("ones_f", [128, 128], F32, outer=True)
    xs = [P.sb("xs%d" % i, [128, D], F32, outer=True) for i in range(2)]
    junk = P.sb("junk", [128, D], BF16, outer=True)
    hb = P.sb("hb", [128, D], BF16, outer=True)
    hb2 = P.sb("hb2", [128, D], BF16, outer=True)
    hb3 = P.sb("hb3", [128, D], BF16, outer=True)
    WcG = P.sb("WcG", [128, 8, 1024], BF16, outer=True)
    KxT = P.sb("KxT", [128, 4, 256], BF16, outer=True)
    Vx = P.sb("Vx", [128, 2, 512], BF16, outer=True)
    st = P.sb("st", [128, 12], F32, outer=True)

    hbs = [hb, hb2, hb3]
    POOL_NORM = [False]
    NDEP = 2

    def emit_stats(xs_ap, xs_key, gb, gb_key, k):
        o = 4 * k
        act(P, junk[:], xs_ap, AF.Square, [xs_key], ["junk", ("st0", k)], accum_out=st[:, o:o + 1])
        act(P, st[:, o + 2:o + 3], st[:, o:o + 1], AF.Ln, [("st0", k)], [("st2", k)], bias=EPS, scale=1.0 / D)
        act(P, st[:, o + 3:o + 4], st[:, o + 2:o + 3], AF.Exp, [("st2", k)], [("st3", k)], scale=-0.5)
        stt(P, "pool" if (POOL_NORM[0] and k == 1) else "dve", hbs[k][:], xs_ap, st[:, o + 3:o + 4], gb[:],
            ALU.mult, ALU.mult, [xs_key, ("st3", k), gb_key], [("hb", k)])

    def emit_T(k, hT, hT_key, col0, evac_eng):
        bk, bkey = P.bank("any", ALLB)
        tpb = bk[:].bitcast(BF16)
        for c in range(8):
            tr(P, tpb[:, c * 128:(c + 1) * 128], hbs[k][:, c * 128:(c + 1) * 128], idb[:], [("hb", k), "idb"], [bkey])
        cp(P, evac_eng, hT[:, :, col0:col0 + 128], tpb[:, 0:1024].rearrange("p (c n) -> p c n", c=8), [bkey], [hT_key])

    ncnt = [0]

    def emit_norm(xs_ap, xs_key, gb, gb_key, hT, hT_key, col0, evac_eng):
        k = ncnt[0] % 3
        ncnt[0] += 1
        emit_stats(xs_ap, xs_key, gb, gb_key, k)
        emit_T(k, hT, hT_key, col0, evac_eng)

    def emit_norm_seq(items, gb, gb_key, pre=None):
        n = len(items)
        ks = []
        LAG = 3
        for idx in range(n + NDEP + LAG):
            if idx < n:
                it = items[idx]
                if it[6] is not None:
                    it[6]()
                k = ncnt[0] % 3
                ncnt[0] += 1
                ks.append(k)
                emit_stats(it[0], it[1], gb, gb_key, k)
            if NDEP <= idx < n + NDEP:
                it = items[idx - NDEP]
                emit_T(ks[idx - NDEP], it[2], it[3], it[4], it[5])
            if pre is not None and idx >= NDEP + LAG:
                pre(idx - NDEP - LAG)

    sel_d = din("sel", [128, 4 * 128])
    NB = 32
    NH = 4
    P.push()
    KT = P.sb("KT", [128, 2, 4096], BF16)
    QTz = P.sb("QTz", [128, NH, 4096], BF16)
    Vp = P.sb("Vp", [128, NB, 256], BF16)
    Qaug = P.sb("Qaug", [128, 4096], BF16)
    Sel = P.sb("Sel", [128, NH, 128], BF16)
    zt = P.sb("zt", [128, NB, NH], F32)
    flag = P.sb("flag", [128, 2], F32)
    maskb = P.sb("maskb", [128, 128], BF16)
    attnO = P.sb("attnO", [128, 2, 4096], BF16)

    P.phase_begin()
    Wq = P.sb("Wqkvf", [128, 8, 772], BF16)
    hT = [P.sb("hT%d" % i, [128, 8, 512], BF16) for i in range(2)]
    gmix = P.sb("gmix", [128, D], F32)
    bfb = P.sb("bfb", [128, NH], F32)

    P.dma("pool", idb[:], ident_d, "c_idb", writes=["idb"])
    P.dma("sp", gmix[:], g_mix_d, "c_gmix", writes=["gmix"])
    P.dma("sp", bfb[:], bf_d, "c_bfb", writes=["bfb"])
    P.dma("pool", Wq[:, :, 512:772], w_own[:, 512:772].rearrange("(c p) n -> p c n", p=128), "w_qv", writes=["WqV"])
    P.dma("pool", Wq[:, :, 256:512], w_own[:, 256:512].rearrange("(c p) n -> p c n", p=128), "w_qk", writes=["WqK"])
    P.dma("pool", Wq[:, :, 0:256], w_own[:, 0:256].rearrange("(c p) n -> p c n", p=128), "w_qq", writes=["WqQ"])
    P.dma("pool", maskb[:], mask_d, "c_mask", writes=["maskb"])
    P.dma("pool", Sel[:].rearrange("p a b -> p (a b)"), sel_d, "c_sel", writes=["Sel"])
    P.dma("sp", flag[:, 0:1], flag_d, "c_flag", writes=["flag"])
    memset(P, "pool", ones_bf[:], 1.0, ["ones_bf"])
    memset(P, "pool", ones_f[:], 1.0, ["ones_f"])
    memset(P, "pool", QTz[:].rearrange("p a b -> p (a b)"), 0.0, ["QTz0"])
    memset(P, "pool", Qaug[:], 0.0, ["Qaug0"])

    def a_load(blk):
        P.dma("sp", xs[blk % 2][:], x_all[blk * 128:(blk + 1) * 128, :], "xs%d" % (blk % 2), writes=[("xs", blk % 2)])

    ev = [0]

    def a_post(blk):
        ch, t = blk // 4, blk % 4
        hTc = hT[ch % 2]
        hkey = ("hT", ch % 2)
        vb, vkey = P.bank("any", ALLB)
        fb, fkey = P.bank("any", ALLB)
        for c in range(8):
            mm(P, vb[:, 0:256], hTc[:, c, t * 128:(t + 1) * 128], Wq[:, c, 512:768], c == 0, c == 7,
               [hkey, "WqV"], [vkey])
        for c in range(8):
            mm(P, fb[:, 0:NH], hTc[:, c, t * 128:(t + 1) * 128], Wq[:, c, 768:772], c == 0, c == 7,
               [hkey, "WqV"], [fkey])
        cp(P, "act", Vp[:, blk, :], vb[:, 0:256], [vkey], [("Vp", blk)])
        tt(P, "dve", zt[:, blk, :], fb[:, 0:NH], bfb[:], ALU.add, [fkey, "bfb"], [("zt", blk)])
        if t != 3:
            return
        for p in range(2):
            kb, kkey = P.bank("any", ALLB)
            for c in range(8):
                mm(P, kb[:, :], Wq[:, c, 256 + p * 128:256 + (p + 1) * 128], hTc[:, c, :], c == 0, c == 7,
                   [hkey, "WqK"], [kkey])
            cp(P, "act" if ev[0] % 2 else "dve", KT[:, p, ch * 512:(ch + 1) * 512], kb[:, :], [kkey], [("KT", p, ch)])
            ev[0] += 1
        for p in range(2):
            qb, qkey = P.bank("any", ALLB)
            for c in range(8):
                mm(P, qb[:, :], Wq[:, c, p * 128:(p + 1) * 128], hTc[:, c, :], c == 0, c == 7,
                   [hkey, "WqQ"], [qkey])
            P.op("act", lambda h, o=QTz[0:64, 2 * p, ch * 512:(ch + 1) * 512], i=qb[0:64, :]: h.mul(o, i, 0.125),
                 [qkey, "QTz0"], [("QTz", 2 * p, ch)])
            ts(P, "dve", QTz[64:128, 2 * p + 1, ch * 512:(ch + 1) * 512], qb[64:128, :], 0.125, None, ALU.mult, None,
               [qkey, "QTz0"], [("QTz", 2 * p + 1, ch)])

    items = []
    for blk in range(NB):
        ch, t = blk // 4, blk % 4
        items.append((xs[blk % 2][:], ("xs", blk % 2), hT[ch % 2], ("hT", ch % 2), t * 128,
                      "act" if blk % 2 else "dve", (lambda blk=blk: a_load(blk))))
    emit_norm_seq(items, gmix, "gmix", pre=a_post)
    P.phase_end()

    P.phase_begin()
    NZ = NB * NH
    scA = P.sb("scA", [128, NB, NH], F32)
    scB = P.sb("scB", [128, NB, NH], F32)
    excl = P.sb("excl", [128, NB, NH], F32)
    ctok = P.sb("ctok", [128, NB, NH], F32)
    cendx = P.sb("cendx", [128, NB + 1, NH], F32)
    biasT = P.sb("biasT", [128, 8, NB, NH], F32)
    lq = P.sb("lq", [128, NB, NH], F32)
    lqhi = P.sb("lqhi", [128, NB, NH], BF16)
    lqlo = P.sb("lqlo", [128, NB, NH], BF16)
    Lst = P.sb("Lst", [128, NB, NH, 2], BF16)
    uneg = P.sb("uneg", [128, 128], F32)
    oneg = P.sb("oneg", [128, 128], F32)
    e127 = P.sb("e127", [128, 128], F32)
    Pt = [P.sb("Pt%d" % i, [128, 512], BF16) for i in range(4)]
    Et = [P.sb("Et%d" % i, [128, 512], F32) for i in range(2)]
    Tm = [P.sb("Tm%d" % i, [128, 512], F32) for i in range(2)]
    rd = P.sb("rd", [128, 512], F32)
    Vh = [P.sb("Vh%d" % i, [128, NB, 128], BF16) for i in range(2)]

    P.dma("sp", uneg[:], uneg_d, "c_uneg", writes=["uneg"])
    P.dma("sp", e127[:], e127_d, "c_e127", writes=["e127"])
    P.dma("pool", WcG[:], w_in[:, 0:1024].rearrange("(c p) n -> p c n", p=128), "w_cg")
    memset(P, "pool", oneg[:], -1.0, ["oneg"])
    memset(P, "pool", excl[:, 0, :], 0.0, ["excl0"])
    memset(P, "pool", cendx[:, 0, :], 0.0, ["cendx0"])
    for b_ in range(2):
        memset(P, "pool", Vh[b_][:, :, 64:128], 1.0, [("Vh1", b_)])

    fl = lambda t_: t_[:].rearrange("p a b -> p (a b)")
    ztf = fl(zt)
    act(P, fl(scA), ztf, AF.Exp, [], ["scA"], scale=-1.0)
    act(P, ztf, fl(scA), AF.Ln, ["scA"], ["nl"], bias=1.0)
    srcT, dstT = zt, scA
    skey, dkey = "nl", "scA"
    for s_ in (1, 2, 4, 8, 16):
        tt(P, "dve", dstT[:, s_:, :], srcT[:, s_:, :], srcT[:, :NB - s_, :], ALU.add, [skey], [dkey])
        cp(P, "dve", dstT[:, :s_, :], srcT[:, :s_, :], [skey], [dkey])
        if s_ == 1:
            srcT, dstT = scA, scB
            skey, dkey = "scA", "scB"
        else:
            srcT, dstT = dstT, srcT
            skey, dkey = dkey, skey
    cp(P, "dve", excl[:, 1:, :], srcT[:, :NB - 1, :], [skey], ["excl"])
    cb, ckey = P.bank("any", ALLB)
    mm(P, cb[:, 0:NZ], uneg[:], ztf, True, False, ["uneg", "nl"], [ckey])
    mm(P, cb[:, 0:NZ], oneg[:], fl(excl), False, True, ["oneg", "excl", "excl0"], [ckey])
    cp(P, "dve", fl(ctok), cb[:, 0:NZ], [ckey], ["ctok"])
    eb, ekey = P.bank("any", ALLB)
    mm(P, eb[:, 0:NZ], e127[:], fl(ctok), True, True, ["e127", "ctok"], [ekey])
    cp(P, "dve", cendx[:, 1:, :].rearrange("p a b -> p (a b)"), eb[:, 0:NZ], [ekey, "cendx0"], ["cendx"])
    cref = cendx[:, 0:NB:4, :]
    tt(P, "dve", biasT[:], cref.unsqueeze(2).to_broadcast([128, 8, NB, NH]),
       ctok[:].unsqueeze(1).to_broadcast([128, 8, NB, NH]), ALU.subtract, ["ctok", "cendx"], ["biasT"])
    tt(P, "dve", lq[:].rearrange("p (i t) h -> p i t h", i=8), ctok[:].rearrange("p (i t) h -> p i t h", i=8),
       cref.unsqueeze(2).to_broadcast([128, 8, 4, NH]), ALU.subtract, ["ctok", "cendx"], ["lq"])
    cp(P, "dve", lqhi[:], lq[:], ["lq"], ["lqhi"])
    tt(P, "dve", lqlo[:], lq[:], lqhi[:], ALU.subtract, ["lq", "lqhi"], ["lqlo"])
    cp(P, "dve", Lst[:, :, :, 0], lqhi[:], ["lqhi"], ["Lst"])
    cp(P, "dve", Lst[:, :, :, 1], lqlo[:], ["lqlo"], ["Lst"])
    for i in range(8):
        tb, tkey = P.bank("any", ALLB)
        tpb = tb[:].bitcast(BF16)
        for t in range(4):
            tr(P, tpb[0:2 * NH, t * 128:(t + 1) * 128], Lst[:, 4 * i + t].rearrange("p a b -> p (a b)"), idb[:],
               ["Lst"], [tkey])
        cp(P, "dve", Qaug[0:2 * NH, i * 512:(i + 1) * 512], tpb[0:2 * NH, 0:512], [tkey, "Qaug0"], [("Qaug", i)])

    SB_IDS = [0, 1, 2, 3]
    O_IDS = [4, 5, 6, 7]
    NPT = 4
    SKEW = 2
    pcount = 0
    ecount = 0
    for h_ in range(NH):
        p = h_ // 2
        r0 = (h_ % 2) * 64
        vh = Vh[h_ % 2]
        vhk = ("Vh", h_ % 2)
        if h_ == 0:
            cp(P, "dve", vh[:, :, 0:64], Vp[:, :, 0:64], [], [vhk])
        for i in range(8):
            if i == 2 and h_ + 1 < NH:
                cp(P, "dve", Vh[(h_ + 1) % 2][:, :, 0:64], Vp[:, :, (h_ + 1) * 64:(h_ + 2) * 64], [],
                   [("Vh", (h_ + 1) % 2)])
            noff = 4 * i
            nj = noff + 4
            qcols = slice(i * 512, (i + 1) * 512)
            odg, odgk = P.bank("O", O_IDS)
            if noff > 0:
                ooff, ooffk = P.bank("O", O_IDS)
                ebk, ebkey = P.bank("S", SB_IDS)
                mm(P, ebk[:, :], Sel[:, h_, :], Qaug[:, qcols], True, True, [("Qaug", i)], [ebkey])
                et = Et[ecount % 2]
                etk = ("Et", ecount % 2)
                act(P, et[:], ebk[:, :], AF.Exp, [ebkey], [etk])
            ecount += 1
            pend = []

            def emit_pv(j, q0, ptile, pkey):
                if j < noff:
                    mm(P, ooff[:, :], vh[:, j, :], ptile[:, :], j == 0, j == noff - 1,
                       [vhk, ("Vh1", h_ % 2), pkey], [ooffk])
                else:
                    mm(P, odg[:, q0:512], vh[:, j, :], ptile[:, q0:512], j == noff, j == nj - 1,
                       [vhk, ("Vh1", h_ % 2), pkey], [odgk])

            for j in range(nj):
                r = j - noff
                q0 = r * 128 if r > 0 else 0
                sbk, skey_ = P.bank("S", SB_IDS)
                diag = r >= 0
                mm(P, sbk[:, q0:512], KT[:, p, j * 128:(j + 1) * 128],
                   QTz[:, h_, i * 512 + q0:(i + 1) * 512], True, not diag, [], [skey_])
                if diag:
                    mm(P, sbk[:, q0:512], Sel[:, h_, :], Qaug[:, i * 512 + q0:(i + 1) * 512], False, True,
                       [("Qaug", i)], [skey_])
                if len(pend) >= SKEW:
                    emit_pv(*pend.pop(0))
                ptile = Pt[pcount % NPT]
                pkey = ("Pt", pcount % NPT)
                pcount += 1
                act(P, ptile[:, q0:512], sbk[:, q0:512], AF.Exp, [skey_, "biasT"], [pkey],
                    bias=biasT[:, i, j, h_:h_ + 1])
                if diag:
                    tt(P, "pool", ptile[:, q0:q0 + 128], ptile[:, q0:q0 + 128], maskb[:], ALU.mult,
                       [pkey], [pkey])
                pend.append((j, q0, ptile, pkey))
            while pend:
                emit_pv(*pend.pop(0))
            tm = Tm[ecount % 2]
            tmk = ("Tm", ecount % 2)
            if noff > 0:
                tt(P, "dve", tm[:], ooff[:, :], et[:], ALU.mult, [ooffk, etk], [tmk])
                tt(P, "dve", tm[:], tm[:], odg[:, :], ALU.add, [tmk, odgk], [tmk])
            else:
                cp(P, "dve", tm[:], odg[:, :], [odgk], [tmk])
            P.op("dve", lambda h, o=rd[0:64, :], i_=tm[64:128, :]: h.reciprocal(o, i_), [tmk], ["rd"])
            tt(P, "dve", attnO[r0:r0 + 64, p, qcols], tm[0:64, :], rd[0:64, :], ALU.mult,
               [tmk, "rd"], [("attnO", p)])
        if h_ % 2 == 1:
            P.dma("sp", ag_in[p * 128:(p + 1) * 128, :], attnO[:, p, :], "ag_w%d" % p, reads=[("attnO", p)],
                  writes=[("ag_in", p)])
    P.phase_end()

    P.phase_begin()
    tA = P.sb("tA", [128, 4, TOWN], BF16)
    tB = P.sb("tB", [128, 4, TOWN], BF16)
    P.collective("AllGather", [ag_in.opt()], [ag_out.opt()], [[0, 1], [2, 3], [4, 5], [6, 7]], [], ["ag_out"])
    Wxk = P.sb("Wxk", [128, 8, 512], BF16)
    Wxv = P.sb("Wxv", [128, 8, 512], BF16)
    gmem = P.sb("gmem", [128, D], F32)
    hmT = P.sb("hmT", [128, 8, 256], BF16)
    P.dma("pool", Wxk[:], xk_w.rearrange("(c p) n -> p c n", p=128), "w_xk", writes=["Wxk"])
    P.dma("pool", Wxv[:], xv_w.rearrange("(c p) n -> p c n", p=128), "w_xv", writes=["Wxv"])
    P.dma("sp", gmem[:], g_mem_d, "c_gmem", writes=["gmem"])
    for mb in range(2):
        P.dma("sp", xs[mb][:], mem[mb * 128:(mb + 1) * 128, :], "xs%d" % mb, writes=[("xs", mb)])
        emit_norm(xs[mb][:], ("xs", mb), gmem, "gmem", hmT, "hmT", mb * 128, "dve")
    for h_ in range(4):
        kb, kkey = P.bank("any", ALLB)
        for c in range(8):
            mm(P, kb[:, 0:256], Wxk[:, c, h_ * 128:(h_ + 1) * 128], hmT[:, c, :], c == 0, c == 7, ["Wxk", "hmT"], [kkey])
        cp(P, "dve", KxT[:, h_, :], kb[:, 0:256], [kkey], ["KxT"])
    for mb in range(2):
        vb, vkey = P.bank("any", ALLB)
        for c in range(8):
            mm(P, vb[:, :], hmT[:, c, mb * 128:(mb + 1) * 128], Wxv[:, c, :], c == 0, c == 7, ["Wxv", "hmT"], [vkey])
        cp(P, "act", Vx[:, mb, :], vb[:, :], [vkey], ["Vx"])
    P.dma("sp", tA[:], ag_out[:, 0:TOWN].rearrange("(a q) n -> q a n", q=128), "ag_rA", reads=["ag_out"], writes=["tA"])
    P.dma("sp", tB[:], ag_out[:, TOWN:2 * TOWN].rearrange("(a q) n -> q a n", q=128), "ag_rB", reads=["ag_out"], writes=["tB"])
    ts(P, "dve", flag[:, 1:2], flag[:, 0:1], -1.0, 1.0, ALU.mult, ALU.add, [], ["fl2"])
    fl2 = lambda t_: t_[:].rearrange("p a b -> p (a b)")
    ts(P, "dve", fl2(tA), fl2(tA), flag[:, 1:2], None, ALU.mult, None, ["tA", "fl2"], ["tA"])
    stt(P, "dve", fl2(attnT), fl2(tB), flag[:, 0:1], fl2(tA), ALU.mult, ALU.add, ["tA", "tB"], ["attnT"])
    if DEBUG:
        P.barrier()
        dump("attnT", attnT[:].rearrange("p a b -> p (a b)"), [128, 4 * TOWN])
    P.phase_end()
    P.pop()

    P.phase_begin()
    Wc = P.sb("Wc", [128, 8, 2048], BF16)
    Wpw = P.sb("Wpw", [128, 4, D], BF16)
    Wab = P.sb("Wab", [128, 4, D], BF16)
    Wo = P.sb("Wout", [128, 8, D], BF16)
    gmix2 = P.sb("gmix2", [128, D], F32)
    dww = P.sb("dww", [128, 4, 31], F32)
    dwb = P.sb("dwb", [128, 4], F32)
    lng = P.sb("lng", [128, 4], F32)
    lnb = P.sb("lnb", [128, 4], F32)
    hTa = P.sb("hTa", [128, 8, 512], BF16)
    uT = P.sb("uT", [128, 4, 544], BF16)
    Dg = P.sb("Dg", [128, 31, 128], BF16)
    lnA = [P.sb("lnA%d" % i, [128, 512], BF16) for i in range(2)]
    lnB = [P.sb("lnB%d" % i, [128, 512], BF16) for i in range(2)]
    mgA = [P.sb("mgA%d" % i, [128, 512], BF16) for i in range(2)]
    cvb = P.sb("cvb", [128, 4, 512], BF16)
    sqb = P.sb("sqb", [128, 4, 512], BF16)
    su = P.sb("su", [128, 4, 512], BF16)
    sgc = P.sb("sgc", [128, 8, 512], BF16)
    sga = P.sb("sga", [128, 8, 512], BF16)
    mg = P.sb("mg", [128, 8, 512], BF16)
    t1 = P.sb("t1", [128, 512], F32)
    t2 = P.sb("t2", [128, 512], F32)
    t3 = P.sb("t3", [128, 512], F32)
    t4 = P.sb("t4", [128, 512], F32)
    mean_s = P.sb("mean_s", [128, 512], F32)
    rstd_s = P.sb("rstd_s", [128, 512], F32)
    oneS = P.sb("oneS", [128, 128], BF16)
    xr = [P.sb("xr%d" % i, [128, D], F32) for i in range(2)]

    P.dma("pool", Wc[:, :, 0:1024], w_in[:, 2568:3592].rearrange("(c p) n -> p c n", p=128), "w_c", writes=["Wc"])
    P.dma("pool", Wc[:, :, 1024:2048], w_in[:, 3592:4616].rearrange("(c p) n -> p c n", p=128), "w_c", writes=["Wc"])
    P.dma("pool", Wpw[:], pw_w.rearrange("(c p) n -> p c n", p=128), "w_pw", writes=["Wpw"])
    P.dma("pool", Wab[:], ab_w.rearrange("(c p) n -> p c n", p=128), "w_ab", writes=["Wab"])
    P.dma("pool", Wo[:], wout_w.rearrange("(c p) n -> p c n", p=128), "w_out", writes=["Wout"])
    P.dma("sp", gmix2[:], g_mix_d, "c_gmix2", writes=["gmix2"])
    P.dma("sp", dww[:].rearrange("p a b -> p (a b)"), dww_d, "c_dww", writes=["dww"])
    P.dma("sp", dwb[:], dwb_d, "c_dwb", writes=["dwb"])
    P.dma("sp", lng[:], lng_d, "c_lng", writes=["lng"])
    P.dma("sp", lnb[:], lnb_d, "c_lnb", writes=["lnb"])
    memset(P, "pool", oneS[:], 1.0 / 512, ["oneS"])

    def glu_tiles(ncol, dst_col0):
        for m in range(4):
            ab_, akey = P.bank("any", ALLB)
            gb_, gkey = P.bank("any", ALLB)
            for c in range(8):
                mm(P, ab_[:, 0:ncol], WcG[:, c, m * 128:(m + 1) * 128], hTa[:, c, 0:ncol], c == 0, c == 7,
                   ["hTa"], [akey])
            for c in range(8):
                mm(P, gb_[:, 0:ncol], WcG[:, c, 512 + m * 128:512 + (m + 1) * 128], hTa[:, c, 0:ncol], c == 0, c == 7,
                   ["hTa"], [gkey])
            tg = (t1, t2)[m % 2]
            tgk = ("tg", m % 2)
            act(P, tg[:, 0:ncol], gb_[:, 0:ncol], AF.Sigmoid, [gkey], [tgk])
            tt(P, "dve", uT[:, m, dst_col0:dst_col0 + ncol], ab_[:, 0:ncol], tg[:, 0:ncol], ALU.mult,
               [akey, tgk], [("uT", m)])

    P.dma("sp", xs[0][:], x_halo, "xs0", writes=[("xs", 0)])
    emit_norm(xs[0][:], ("xs", 0), gmix2, "gmix2", hTa, "hTa", 0, "dve")
    for m in range(4):
        memset(P, "pool", uT[:, m, 0:32], 0.0, [("uT", m)])
    glu_tiles(128, 32)
    for m in range(4):
        cp(P, "dve", uT[:, m, 2:32], uT[:, m, 130:160], [("uT", m)], [("uT", m)])

    def emit_gates(i, f):
        gcb, gckey = P.bank("any", ALLB)
        gab, gakey = P.bank("any", ALLB)
        for c in range(8):
            mm(P, gcb[:, :], Wc[:, c, f * 128:(f + 1) * 128], hTa[:, c, :], c == 0, c == 7,
               ["hTa", "Wc"], [gckey])
        for c in range(8):
            mm(P, gab[:, :], Wc[:, c, 1024 + f * 128:1024 + (f + 1) * 128], hTa[:, c, :], c == 0, c == 7,
               ["hTa", "Wc"], [gakey])
        act(P, sgc[:, f, :], gcb[:, :], AF.Sigmoid, [gckey], [("sgc", f)])
        act(P, sga[:, f, :], gab[:, :], AF.Sigmoid, [gakey], [("sga", f)])

    def ca_front(i):
        items = []
        for t in range(4):
            blk = i * 4 + t
            k2 = blk % 2

            def ld(blk=blk, k2=k2):
                P.dma("sp", xs[k2][:], x_own[blk * 128:(blk + 1) * 128, :], "xs%d" % k2, writes=[("xs", k2)])

            items.append((xs[k2][:], ("xs", k2), hTa, "hTa", t * 128, "act" if t % 2 else "dve", ld))
        emit_norm_seq(items, gmix2, "gmix2")

    ca_front(0)
    for i in range(4):
        def gen_dg(m):
            tt(P, "dve", Dg[:], idb[:].unsqueeze(1).to_broadcast([128, 31, 128]),
               dww[:, m, :].unsqueeze(2).to_broadcast([128, 31, 128]), ALU.mult, ["dww"], ["Dg"])

        gen_dg(0)
        glu_tiles(512, 32)
        for m in range(4):
            if m > 0:
                gen_dg(m)
            cb_, ckey_ = P.bank("any", ALLB)
            for w in range(31):
                mm(P, cb_[:, :], Dg[:, w, :], uT[:, m, 2 + w:2 + w + 512], w == 0, w == 30, ["Dg", ("uT", m)], [ckey_])
            act(P, cvb[:, m, :], cb_[:, :], AF.Identity, [ckey_, "dwb"], [("cvb", m)], bias=dwb[:, m:m + 1])
            act(P, sqb[:, m, :], cb_[:, :], AF.Square, [ckey_, "dwb"], [("sqb", m)], bias=dwb[:, m:m + 1])
            cp(P, "pool", uT[:, m, 2:32], uT[:, m, 514:544], [("uT", m)], [("uT", m)])
            emit_gates(i, 2 * m)
            emit_gates(i, 2 * m + 1)
        mb_, mkey = P.bank("any", ALLB)
        qb_, qkey_ = P.bank("any", ALLB)
        for m in range(4):
            mm(P, mb_[:, :], oneS[:], cvb[:, m, :], m == 0, m == 3, ["oneS", ("cvb", m)], [mkey])
        for m in range(4):
            mm(P, qb_[:, :], oneS[:], sqb[:, m, :], m == 0, m == 3, ["oneS", ("sqb", m)], [qkey_])
        ln_ops = []
        ln_ops.append(lambda: cp(P, "act", mean_s[:], mb_[:, :], [mkey], ["mean_s"]))
        ln_ops.append(lambda: tt(P, "dve", t2[:], mean_s[:], mean_s[:], ALU.mult, ["mean_s"], ["t2"]))
        ln_ops.append(lambda: tt(P, "dve", t3[:], qb_[:, :], t2[:], ALU.subtract, [qkey_, "t2"], ["t3"]))
        ln_ops.append(lambda: act(P, t2[:], t3[:], AF.Ln, ["t3"], ["t2"], bias=EPS))
        ln_ops.append(lambda: act(P, rstd_s[:], t2[:], AF.Exp, ["t2"], ["rstd_s"], scale=-0.5))
        ln_ops.append(lambda: stt(P, "dve", t4[:], mean_s[:], -1.0, rstd_s[:], ALU.mult, ALU.mult,
                                  ["mean_s", "rstd_s"], ["nb"]))

        def ln_m(m):
            ta = lnA[m % 2]
            tb_ = lnB[m % 2]
            tt(P, "dve", ta[:], cvb[:, m, :], rstd_s[:], ALU.mult, [("cvb", m), "rstd_s"], [("lnA", m % 2)])
            tt(P, "dve", tb_[:], ta[:], t4[:], ALU.add, [("lnA", m % 2), "nb"], [("lnB", m % 2)])
            act(P, su[:, m, :], tb_[:], AF.Silu, [("lnB", m % 2), "lng", "lnb"], [("su", m)], bias=lnb[:, m:m + 1],
                scale=lng[:, m:m + 1])

        for m in range(4):
            ln_ops.append(lambda m=m: ln_m(m))

        def ab_f(f):
            aob, aokey = P.bank("any", ALLB)
            for c in range(4):
                mm(P, aob[:, :], Wab[:, c, f * 128:(f + 1) * 128], attnT[:, c, i * 512:(i + 1) * 512], c == 0, c == 3,
                   ["Wab"], [aokey])
            tt(P, "dve", mg[:, f, :], aob[:, :], sga[:, f, :], ALU.mult, [aokey, ("sga", f)], [("mg", f)])

        ab_ops = [(lambda f=f: ab_f(f)) for f in range(8)]
        while ln_ops or ab_ops:
            if ab_ops:
                ab_ops.pop(0)()
            if ln_ops:
                ln_ops.pop(0)()
        for f in range(8):
            cob, cokey = P.bank("any", ALLB)
            for c in range(4):
                mm(P, cob[:, :], Wpw[:, c, f * 128:(f + 1) * 128], su[:, c, :], c == 0, c == 3,
                   ["Wpw", ("su", c)], [cokey])
            ma = mgA[f % 2]
            tt(P, "dve", ma[:], cob[:, :], sgc[:, f, :], ALU.mult, [cokey, ("sgc", f)], [("mgA", f % 2)])
            tt(P, "dve", mg[:, f, :], mg[:, f, :], ma[:], ALU.add, [("mgA", f % 2), ("mg", f)], [("mg", f)])
        if i + 1 < 4:
            ca_front(i + 1)
        for t in range(4):
            blk = i * 4 + t
            xb = xr[t % 2]
            xk = ("xr", t % 2)
            P.dma("sp", xb[:], x_own[blk * 128:(blk + 1) * 128, :], "xr%d" % (t % 2), writes=[xk])
            for n in range(2):
                db, dkey_ = P.bank("any", ALLB)
                for c in range(8):
                    mm(P, db[:, :], mg[:, c, t * 128:(t + 1) * 128], Wo[:, c, n * 512:(n + 1) * 512], c == 0, c == 7,
                       [("mg", c), "Wout"], [dkey_])
                tt(P, "dve", xb[:, n * 512:(n + 1) * 512], xb[:, n * 512:(n + 1) * 512], db[:, :], ALU.add,
                   [xk, dkey_], [xk])
            P.dma("sp", x1_d[blk * 128:(blk + 1) * 128, :], xb[:], "x1w%d" % (t % 2), reads=[xk], writes=[("x1", blk)])
    P.phase_end()

    P.push()
    xacc = P.sb("xacc", [128, 16, D], F32)
    h2T = P.sb("h2T", [128, 8, TOWN], BF16)
    comb = P.sb("comb", [128, 16, 16], F32)
    sa = [P.sb("sa%d" % i, [128, 512], BF16) for i in range(2)]
    uu = [P.sb("uu%d" % i, [128, 512], BF16) for i in range(4)]

    def emit_expert_sub(e, s, wgf, wdf, wkeys):
        hk = ("h2T", s)
        for m in range(2):
            ab_, akey = P.bank("any", ALLB)
            bb_, bkey = P.bank("any", ALLB)
            for c in range(8):
                mm(P, ab_[:, :], wgf(c, m * 128, (m + 1) * 128), h2T[:, c, s * 512:(s + 1) * 512], c == 0, c == 7,
                   wkeys + [hk], [akey])
            for c in range(8):
                mm(P, bb_[:, :], wgf(c, 256 + m * 128, 256 + (m + 1) * 128), h2T[:, c, s * 512:(s + 1) * 512],
                   c == 0, c == 7, wkeys + [hk], [bkey])
            act(P, sa[m][:], ab_[:, :], AF.Silu, [akey], [("sa", m)])
            tt(P, "dve", uu[(s % 2) * 2 + m][:], bb_[:, :], sa[m][:], ALU.mult, [bkey, ("sa", m)],
               [("uu", (s % 2) * 2 + m)])
        for t in range(4):
            lb = s * 4 + t
            for n in range(2):
                yb, ykey = P.bank("any", ALLB)
                for m in range(2):
                    mm(P, yb[:, :], uu[(s % 2) * 2 + m][:, t * 128:(t + 1) * 128], wdf(m, n),
                       m == 0, m == 1, [("uu", (s % 2) * 2 + m)] + wkeys, [ykey])
                stt(P, "dve", xacc[:, lb, n * 512:(n + 1) * 512], yb[:, :], comb[:, lb, e:e + 1],
                    xacc[:, lb, n * 512:(n + 1) * 512], ALU.mult, ALU.add,
                    [ykey, ("comb", lb // 4), ("xacc", lb)], [("xacc", lb)])

    wgf0 = lambda c, lo, hi: WcG[:, c, lo:hi]
    wdf0 = lambda m, n: WcG[:, 2 * m + n, 512:1024]

    P.phase_begin()
    Wxq = P.sb("Wxq", [128, 8, 512], BF16)
    Wxo = P.sb("Wxo", [128, 4, D], BF16)
    gxa = P.sb("gxa", [128, D], F32)
    h1T = P.sb("h1T", [128, 8, 512], BF16)
    qxT = P.sb("qxT", [128, 4, 512], BF16)
    Px = [P.sb("Px%d" % i, [128, 512], BF16) for i in range(4)]
    oxTs = [P.sb("oxT%d" % i, [128, 4, 512], BF16) for i in range(2)]
    rdx = P.sb("rdx", [128, 512], F32)
    P.dma("pool", Wxq[:], xq_w.rearrange("(c p) n -> p c n", p=128), "w_xq", writes=["Wxq"])
    P.dma("pool", Wxo[:], xo_w.rearrange("(c p) n -> p c n", p=128), "w_xo", writes=["Wxo"])
    P.dma("sp", gxa[:], g_xa_d, "c_gxa", writes=["gxa"])
    P.dma("pool", WcG[:, :, 0:256], eg_w[0].rearrange("(c p) n -> p c n", p=128), "w_e0p", writes=["We0"])
    P.dma("pool", WcG[:, :, 256:512], eu_w[0].rearrange("(c p) n -> p c n", p=128), "w_e0p", writes=["We0"])
    for m_ in range(2):
        P.dma("pool", WcG[:, 2 * m_:2 * m_ + 2, 512:1024],
              ed_w[0][m_ * 128:(m_ + 1) * 128, :].rearrange("p (n f) -> p n f", n=2), "w_e0p", writes=["We0"])
    Wr = P.sb("Wr", [128, 8, 20], BF16)
    rbb = P.sb("rbb", [128, 20], F32)
    gmoe = P.sb("gmoe", [128, D], F32)
    rL = P.sb("rL", [128, 4, 20], F32)
    rS = P.sb("rS", [128, 6, 4], F32)
    rG = P.sb("rG", [128, 3, 4, 4], F32)
    rE = P.sb("rE", [128, 3, 4, 16], F32)
    P.dma("pool", Wr[:], wr_d.rearrange("(c p) n -> p c n", p=128), "w_r", writes=["Wr"])
    P.dma("sp", rbb[:], rb_d, "c_rbb", writes=["rbb"])
    P.dma("sp", gmoe[:], g_moe_d, "c_gmoe", writes=["gmoe"])
    BIG = 30000.0

    def emit_moe_front(lb0):
        emit_moe_norms(lb0)
        emit_moe_router(lb0)

    def emit_moe_norms(lb0):
        for lb in range(lb0, lb0 + 4):
            emit_norm(xacc[:, lb, :], ("xacc", lb), gmoe, "gmoe", h2T, ("h2T", lb // 4), lb * 128, "act" if lb % 2 else "dve")

    def emit_moe_router(lb0):
        rb_, rkey = P.bank("any", ALLB)
        for k in range(4):
            lb = lb0 + k
            for c in range(8):
                mm(P, rb_[:, k * 20:(k + 1) * 20], h2T[:, c, lb * 128:(lb + 1) * 128], Wr[:, c, :], c == 0, c == 7,
                   [("h2T", lb // 4), "Wr"], [rkey])
        AXX = mybir.AxisListType.X
        L = rL[:]
        tt(P, "dve", L, rb_[:, 0:80].rearrange("p (k n) -> p k n", k=4), rbb[:].unsqueeze(1).to_broadcast([128, 4, 20]),
           ALU.add, [rkey, "rbb"], ["rL"])
        P.op("dve", lambda h: h.tensor_reduce(rS[:, 0, :], rL[:, :, 0:4], AXX, ALU.max), ["rL"], ["gmax"])
        tt(P, "dve", rG[:, 0], rL[:, :, 0:4], rS[:, 0, :].unsqueeze(2).to_broadcast([128, 4, 4]), ALU.is_ge,
           ["rL", "gmax"], ["gmask"])
        tt(P, "dve", rG[:, 1], rL[:, :, 0:4], rS[:, 0, :].unsqueeze(2).to_broadcast([128, 4, 4]), ALU.subtract,
           ["rL", "gmax"], ["gsh"])
        act(P, rG[:, 1], rG[:, 1], AF.Exp, ["gsh"], ["geg"])
        P.op("dve", lambda h: h.tensor_reduce(rS[:, 1, :], rG[:, 1], AXX, ALU.add), ["geg"], ["gsum"])
        ts(P, "dve", rG[:, 2], rG[:, 0], BIG, -BIG, ALU.mult, ALU.add, ["gmask"], ["pen"])
        tt(P, "dve", rE[:, 0].rearrange("p k (g e) -> p k g e", g=4),
           rL[:, :, 4:20].rearrange("p k (g e) -> p k g e", g=4),
           rG[:, 2].unsqueeze(3).to_broadcast([128, 4, 4, 4]), ALU.add, ["rL", "pen"], ["lem"])
        P.op("dve", lambda h: h.tensor_reduce(rS[:, 2, :], rE[:, 0], AXX, ALU.max), ["lem"], ["m1"])
        tt(P, "dve", rE[:, 1], rE[:, 0], rS[:, 2, :].unsqueeze(2).to_broadcast([128, 4, 16]), ALU.is_ge,
           ["lem", "m1"], ["mask1"])
        stt(P, "dve", rE[:, 2], rE[:, 1], -BIG, rE[:, 0], ALU.mult, ALU.add, ["mask1", "lem"], ["lem2"])
        P.op("dve", lambda h: h.tensor_reduce(rS[:, 3, :], rE[:, 2], AXX, ALU.max), ["lem2"], ["m2"])
        tt(P, "dve", rE[:, 1], rE[:, 0], rS[:, 3, :].unsqueeze(2).to_broadcast([128, 4, 16]), ALU.is_ge,
           ["lem", "m2", "lem2"], ["selm"])
        tt(P, "dve", rE[:, 2], rE[:, 0], rS[:, 2, :].unsqueeze(2).to_broadcast([128, 4, 16]), ALU.subtract,
           ["lem", "m1", "selm"], ["esh"])
        act(P, rE[:, 2], rE[:, 2], AF.Exp, ["esh"], ["eex"])
        tt(P, "dve", rE[:, 2], rE[:, 2], rE[:, 1], ALU.mult, ["eex", "selm"], ["eex2"])
        P.op("dve", lambda h: h.tensor_reduce(rS[:, 4, :], rE[:, 2], AXX, ALU.add), ["eex2"], ["ssum"])
        tt(P, "dve", rS[:, 5, :], rS[:, 1, :], rS[:, 4, :], ALU.mult, ["gsum", "ssum"], ["coef0"])
        P.op("dve", lambda h: h.reciprocal(rS[:, 5, :], rS[:, 5, :]), ["coef0"], ["coef"])
        tt(P, "dve", comb[:, lb0:lb0 + 4, :], rE[:, 2], rS[:, 5, :].unsqueeze(2).to_broadcast([128, 4, 16]), ALU.mult,
           ["eex2", "coef"], [("comb", lb0 // 4)])


    XS = float(128 ** -0.5)
    pxc = [0]

    def cb_A(s):
        oxT = oxTs[s % 2]
        items = []
        for t in range(4):
            lb = s * 4 + t

            def ld(lb=lb):
                P.dma("sp", xacc[:, lb, :], x1_d[lb * 128:(lb + 1) * 128, :], "xacc%d" % lb, writes=[("xacc", lb)])

            items.append((xacc[:, lb, :], ("xacc", lb), h1T, "h1T", t * 128, "act" if t % 2 else "dve", ld))
        emit_norm_seq(items, gxa, "gxa")
        for h_ in range(4):
            qb, qkey = P.bank("any", ALLB)
            for c in range(8):
                mm(P, qb[:, :], Wxq[:, c, h_ * 128:(h_ + 1) * 128], h1T[:, c, :], c == 0, c == 7, ["Wxq", "h1T"], [qkey])
            P.op("act", lambda h, o=qxT[:, h_, :], i_=qb[:, :]: h.mul(o, i_, XS), [qkey], [("qxT", h_)])
        def x_s1(h_):
            pts = []
            for mb in range(2):
                sb_, skey_ = P.bank("any", ALLB)
                mm(P, sb_[:, :], KxT[:, h_, mb * 128:(mb + 1) * 128], qxT[:, h_, :], True, True,
                   [("qxT", h_)], [skey_])
                ptile = Px[pxc[0] % 4]
                pkey = ("Px", pxc[0] % 4)
                pxc[0] += 1
                act(P, ptile[:], sb_[:, :], AF.Exp, [skey_], [pkey])
                pts.append((ptile, pkey))
            return pts

        def x_s2(h_, pts):
            db, dkey_ = P.bank("any", ALLB)
            ob, okey = P.bank("any", ALLB)
            for mb in range(2):
                mm(P, db[:, :], ones_bf[:], pts[mb][0][:], mb == 0, mb == 1, [pts[mb][1]], [dkey_])
            for mb in range(2):
                mm(P, ob[:, :], Vx[:, mb, h_ * 128:(h_ + 1) * 128], pts[mb][0][:], mb == 0, mb == 1,
                   [pts[mb][1]], [okey])
            act(P, rdx[:], db[:, :], AF.Ln, [dkey_], ["rdx"])
            act(P, rdx[:], rdx[:], AF.Exp, ["rdx"], ["rdx"], scale=-1.0)
            tt(P, "dve", oxT[:, h_, :], ob[:, :], rdx[:], ALU.mult, [okey, "rdx"], [("oxT", s % 2, h_)])

        prev = None
        for h_ in range(4):
            cur = (h_, x_s1(h_))
            if prev is not None:
                x_s2(*prev)
            prev = cur
        x_s2(*prev)

    def cb_B(s, fill=None):
        oxT = oxTs[s % 2]
        for t in range(4):
            lb = s * 4 + t
            for n in range(2):
                wb_, wkey = P.bank("any", ALLB)
                for c in range(4):
                    mm(P, wb_[:, :], oxT[:, c, t * 128:(t + 1) * 128], Wxo[:, c, n * 512:(n + 1) * 512], c == 0, c == 3,
                       [("oxT", s % 2, c), "Wxo"], [wkey])
                tt(P, "dve", xacc[:, lb, n * 512:(n + 1) * 512], xacc[:, lb, n * 512:(n + 1) * 512], wb_[:, :], ALU.add,
                   [("xacc", lb), wkey], [("xacc", lb)])
        if fill is None:
            emit_moe_front(s * 4)
        else:
            fill(0)
            emit_moe_norms(s * 4)
            fill(1)
            emit_moe_router(s * 4)
            fill(2)

    cb_A(0)
    for s in range(4):
        if s + 1 < 4:
            cb_A(s + 1)
        if s == 3:
            cb_B(s, fill=lambda s0: emit_expert_sub(0, s0, wgf0, wdf0, ["We0"]))
        else:
            cb_B(s)
    POOL_NORM[0] = False
    P.phase_end()

    P.phase_begin()
    gfin = P.sb("gfin", [128, D], F32)
    Wgu = [P.sb("Wgu%d" % i, [128, 8, 512], BF16) for i in range(2)]
    Wd = [P.sb("Wd%d" % i, [128, 2, D], BF16) for i in range(2)]
    ot = [P.sb("ot%d" % i, [128, D], F32) for i in range(2)]

    P.dma("sp", gfin[:], g_fin_d, "c_gfin", writes=["gfin"])
    wslot = 0

    def load_expert(e):
        sl = e % 2
        wk = ("We", sl)
        P.dma("pool", Wgu[sl][:, :, 0:256], eg_w[e].rearrange("(c p) n -> p c n", p=128), "w_e%d" % sl, writes=[wk])
        P.dma("pool", Wgu[sl][:, :, 256:512], eu_w[e].rearrange("(c p) n -> p c n", p=128), "w_e%d" % sl, writes=[wk])
        P.dma("pool", Wd[sl][:], ed_w[e].rearrange("(c p) n -> p c n", p=128), "w_e%d" % sl, writes=[wk])

    load_expert(1)
    load_expert(2)
    def emit_final(lb):
            xa_ = xacc[:, lb, :]
            k_ = lb % 2
            o4 = 4 * k_
            act(P, junk[:], xa_, AF.Square, [("xacc", lb)], ["junk", ("st0", k_)], accum_out=st[:, o4:o4 + 1])
            act(P, st[:, o4 + 2:o4 + 3], st[:, o4:o4 + 1], AF.Ln, [("st0", k_)], [("st2", k_)], bias=EPS, scale=1.0 / D)
            act(P, st[:, o4 + 3:o4 + 4], st[:, o4 + 2:o4 + 3], AF.Exp, [("st2", k_)], [("st3", k_)], scale=-0.5)
            o_ = ot[lb % 2]
            stt(P, "dve", o_[:], xa_, st[:, o4 + 3:o4 + 4], gfin[:], ALU.mult, ALU.mult, [("xacc", lb), ("st3", k_), "gfin"], [("ot", lb % 2)])
            P.dma("sp", out_d[lb * 128:(lb + 1) * 128, :], o_[:], "outw%d" % (lb % 2), reads=[("ot", lb % 2)], writes=[("out", lb)])


    for e in range(16):
        sl = e % 2
        wk = ("We", sl)
        if e == 0:
            emit_expert_sub(0, 3, wgf0, wdf0, [])
        else:
            wgf = lambda c, lo, hi, w_=Wgu[sl]: w_[:, c, lo:hi]
            wdf = lambda m, n, w_=Wd[sl]: w_[:, m, n * 512:(n + 1) * 512]
            for s in range(4):
                emit_expert_sub(e, s, wgf, wdf, [wk])
                if e == 15:
                    for lb in range(4 * s, 4 * s + 4):
                        emit_final(lb)
        if e >= 1 and e + 2 < 16:
            load_expert(e + 2)
    P.phase_end()
    P.pop()

    P.outer.close()
    return nc


_NC = None
DEBUG = False


def _consts():
    idx = np.arange(128)
    ident = np.eye(128, dtype=np.float32)
    uneg = -(idx[:, None] <= idx[None, :]).astype(np.float32)
    e127 = np.zeros((128, 128), np.float32)
    e127[127, :] = 1.0
    mask01 = (idx[:, None] <= idx[None, :]).astype(np.float32)
    return ident, uneg, e127, mask01


def _bc(v, n=128):
    v = np.asarray(v, np.float32).reshape(1, -1)
    return np.ascontiguousarray(np.broadcast_to(v, (n, v.shape[1])))


def _col(v, m):
    return np.ascontiguousarray(np.asarray(v, np.float32).reshape(m, 128).T)


def kernel(x, mem, norm_mix_g, w_in, fox_bf, conv_dw_w, conv_dw_b, conv_ln_g, conv_ln_b,
           conv_pw_w, attn_branch_w, w_out, norm_xa_g, norm_mem_g, xa_wq, xa_wk, xa_wv, xa_wo,
           norm_moe_g, router_group_w, router_group_b, router_expert_w, router_expert_b,
           expert_w_gate, expert_w_up, expert_w_down, norm_final_g):
    global _NC
    if _NC is None:
        _NC = build_program()
    nc = _NC
    f = lambda a: np.ascontiguousarray(np.asarray(a, np.float32))
    x = f(x)
    mem = f(mem)
    ident, uneg, e127, mask01 = _consts()
    sel = np.zeros((128, 4, 128), np.float32)
    for hh in range(4):
        sel[2 * hh:2 * hh + 2, hh, :] = 1.0
    sel = sel.reshape(128, 512)
    dw = f(conv_dw_w)[0]
    dw_t = np.ascontiguousarray(dw.reshape(31, 4, 128).transpose(2, 1, 0)).reshape(128, 4 * 31)
    shared = {
        "w_in": f(w_in)[0], "conv_pw_w": f(conv_pw_w)[0], "attn_branch_w": f(attn_branch_w)[0],
        "w_out": f(w_out)[0], "xa_wq": f(xa_wq)[0], "xa_wk": f(xa_wk)[0], "xa_wv": f(xa_wv)[0],
        "xa_wo": f(xa_wo)[0], "expert_w_gate": f(expert_w_gate)[0], "expert_w_up": f(expert_w_up)[0],
        "expert_w_down": f(expert_w_down)[0],
        "wr": np.ascontiguousarray(np.concatenate([f(router_group_w)[0], f(router_expert_w)[0]], axis=1)),
        "rb_b": _bc(np.concatenate([f(router_group_b)[0], f(router_expert_b)[0]])),
        "g_mix_b": _bc(f(norm_mix_g)[0]), "g_xa_b": _bc(f(norm_xa_g)[0]), "g_mem_b": _bc(f(norm_mem_g)[0]),
        "g_moe_b": _bc(f(norm_moe_g)[0]), "g_fin_b": _bc(f(norm_final_g)),
        "dw_w_t": dw_t, "dw_b_t": _col(f(conv_dw_b)[0], 4), "ln_g_t": _col(f(conv_ln_g)[0], 4),
        "ln_b_t": _col(f(conv_ln_b)[0], 4),
        "ident": ident, "uneg": uneg, "e127": e127, "mask01": mask01, "sel": sel,
    }
    in_maps = []
    zeros128 = np.zeros((128, D), np.float32)
    w_in0 = shared["w_in"]
    bf0 = f(fox_bf)[0]
    for c in range(NCORES):
        b, half = c // 2, c % 2
        m = dict(shared)
        m["x_all"] = np.ascontiguousarray(x[b])
        m["x_own"] = np.ascontiguousarray(x[b, half * TOWN:(half + 1) * TOWN])
        m["x_halo"] = np.ascontiguousarray(x[b, TOWN - 128:TOWN]) if half == 1 else zeros128
        m["mem"] = np.ascontiguousarray(mem[b])
        m["flag"] = np.full((128, 1), float(half), np.float32)
        o = 256 * half
        m["w_own"] = np.ascontiguousarray(np.concatenate(
            [w_in0[:, 1024 + o:1024 + o + 256], w_in0[:, 1536 + o:1536 + o + 256],
             w_in0[:, 2048 + o:2048 + o + 256], w_in0[:, 2560 + 4 * half:2560 + 4 * half + 4]], axis=1))
        m["bf_b"] = _bc(bf0[4 * half:4 * half + 4])
        in_maps.append(m)
    if DEBUG:
        return nc, in_maps
    res = run_bass_kernel_spmd(nc, in_maps, core_ids=list(range(NCORES)))
    out = np.empty((4, 2 * TOWN, D), np.float32)
    for c in range(NCORES):
        b, half = c // 2, c % 2
        out[b, half * TOWN:(half + 1) * TOWN] = res.results[c]["out"]
    return out
```

```python
import contextlib
import numpy as np
import concourse.bass as bass
import concourse.mybir as mybir
from concourse.bass_utils import run_bass_kernel_spmd

F32 = mybir.dt.float32
BF16 = mybir.dt.bfloat16
AF = mybir.ActivationFunctionType
ALU = mybir.AluOpType

ENGS = ("pe", "act", "dve", "pool", "sp")
EPS = 1e-6
D = 1024
TOWN = 2048
NCORES = 8
EMBED_WAIT = True


class Prog:
    def __init__(self, nc):
        self.nc = nc
        self.outer = contextlib.ExitStack()
        self.stacks = [self.outer]
        self.ops = {e: [] for e in ENGS}
        self.cnt = {e: 0 for e in ENGS}
        self.esem = {e: self.outer.enter_context(nc.semaphore("s_" + e)) for e in ENGS}
        self.dsem = {}
        self.dcnt = {}
        self.last_w = {}
        self.readers = {}
        self.waited = {e: {} for e in ENGS}
        self.bank_rr = {}

    def push(self):
        self.stacks.append(contextlib.ExitStack())

    def pop(self):
        self.stacks.pop().close()

    def sb(self, name, shape, dt, outer=False):
        st = self.outer if outer else self.stacks[-1]
        return st.enter_context(self.nc.sbuf_tensor("sb_" + name, list(shape), dt))

    def ps(self, name, shape, dt):
        return self.outer.enter_context(self.nc.psum_tensor(name, list(shape), dt))

    def _dsem(self, name):
        if name not in self.dsem:
            self.dsem[name] = self.outer.enter_context(self.nc.semaphore("d_" + name))
            self.dcnt[name] = 0
        return self.dsem[name]

    def _deps(self, eng, reads, writes):
        toks = []
        for k in reads:
            t = self.last_w.get(k)
            if t is not None:
                toks.append(t)
        for k in writes:
            t = self.last_w.get(k)
            if t is not None:
                toks.append(t)
            toks.extend(self.readers.get(k, ()))
        need = {}
        for (sem, val, owner) in toks:
            if owner == eng and eng in ("pe", "sp"):
                continue
            sid = id(sem)
            if self.waited[eng].get(sid, 0) >= val:
                continue
            if sid not in need or need[sid][1] < val:
                need[sid] = (sem, val)
        for sid, (sem, val) in need.items():
            self.waited[eng][sid] = val
        return list(need.values())

    def _commit(self, tok, reads, writes):
        for k in reads:
            self.readers.setdefault(k, []).append(tok)
        for k in writes:
            self.last_w[k] = tok
            self.readers[k] = []

    def op(self, eng, fn, reads=(), writes=()):
        waits = self._deps(eng, reads, writes)
        self.cnt[eng] += 1
        val = self.cnt[eng]
        sem = self.esem[eng]

        def emit(h, fn=fn, waits=waits, sem=sem):
            if EMBED_WAIT and waits:
                for (s, v) in waits[1:]:
                    h.wait_ge(s, v)
                ins = fn(h)
                ins._wait_ge(waits[0][0], waits[0][1])
                ins.then_inc(sem, 1)
                return
            for (s, v) in waits:
                h.wait_ge(s, v)
            fn(h).then_inc(sem, 1)

        self.ops[eng].append(emit)
        self._commit((sem, val, eng), reads, writes)

    def dma(self, q, out, in_, sem_name, reads=(), writes=()):
        waits = self._deps(q, reads, writes)
        sem = self._dsem(sem_name)
        self.dcnt[sem_name] += 16
        val = self.dcnt[sem_name]

        def emit(h, waits=waits, sem=sem, out=out, in_=in_):
            for (s, v) in waits:
                h.wait_ge(s, v)
            h.dma_start(out=out, in_=in_).then_inc(sem, 16)

        self.ops[q].append(emit)
        self._commit((sem, val, "dma:" + sem_name), reads, writes)

    def collective(self, kind, ins, outs, groups, reads, writes):
        waits = self._deps("pool", reads, writes)
        sem = self._dsem("cc")
        self.dcnt["cc"] += 1
        val = self.dcnt["cc"]

        def emit(h, waits=waits, sem=sem):
            for (s, v) in waits:
                h.wait_ge(s, v)
            h.collective_compute(kind, ALU.bypass, replica_groups=groups, ins=ins, outs=outs).then_inc(sem, 1)

        self.ops["pool"].append(emit)
        self._commit((sem, val, "dma:cc"), reads, writes)

    def barrier(self):
        allw = [(self.esem[e], self.cnt[e], e) for e in ENGS if self.cnt[e] > 0]
        allw += [(self.dsem[n], self.dcnt[n], "dma:" + n) for n in self.dsem if self.dcnt[n] > 0]
        for eng in ENGS:
            waits = []
            for (sem, val, owner) in allw:
                if owner == eng:
                    continue
                if self.waited[eng].get(id(sem), 0) >= val:
                    continue
                self.waited[eng][id(sem)] = val
                waits.append((sem, val))

            def emit(h, waits=waits):
                for (s, v) in waits:
                    h.wait_ge(s, v)

            self.ops[eng].append(emit)
        self.last_w = {}
        self.readers = {}

    def phase_begin(self):
        self.push()

    def phase_end(self):
        self.barrier()
        nc = self.nc
        ops = self.ops
        with nc.Block() as block:
            @block.tensor
            def _(h):
                for f in ops["pe"]:
                    f(h)

            @block.scalar
            def _(h):
                for f in ops["act"]:
                    f(h)

            @block.vector
            def _(h):
                for f in ops["dve"]:
                    f(h)

            @block.gpsimd
            def _(h):
                for f in ops["pool"]:
                    f(h)

            @block.sync
            def _(h):
                for f in ops["sp"]:
                    f(h)
        self.ops = {e: [] for e in ENGS}
        self.pop()

    def bank(self, group, ids):
        i = self.bank_rr.get(group, 0)
        self.bank_rr[group] = i + 1
        b = ids[i % len(ids)]
        return self.banks[b], ("bank", b)


def mm(P, out, lhsT, rhs, start, stop, reads, writes):
    P.op("pe", lambda h: h.matmul(out, lhsT, rhs, start=start, stop=stop), reads, writes)


def tr(P, out, in_, ident, reads, writes):
    P.op("pe", lambda h: h.transpose(out, in_, ident), reads, writes)


def act(P, out, in_, func, reads, writes, bias=None, scale=None, accum_out=None):
    kw = {}
    if bias is not None:
        kw["bias"] = bias
    if scale is not None:
        kw["scale"] = scale
    if accum_out is not None:
        kw["accum_out"] = accum_out
    P.op("act", lambda h: h.activation(out, in_, func, **kw), reads, writes)


def tt(P, eng, out, in0, in1, op, reads, writes):
    P.op(eng, lambda h: h.tensor_tensor(out, in0, in1, op), reads, writes)


def ts(P, eng, out, in0, s1, s2, op0, op1, reads, writes):
    if s2 is None:
        P.op(eng, lambda h: h.tensor_scalar(out, in0, s1, None, op0), reads, writes)
    else:
        P.op(eng, lambda h: h.tensor_scalar(out, in0, s1, s2, op0, op1), reads, writes)


def stt(P, eng, out, in0, scalar, in1, op0, op1, reads, writes):
    P.op(eng, lambda h: h.scalar_tensor_tensor(out, in0, scalar, in1, op0, op1), reads, writes)


def cp(P, eng, out, in_, reads, writes):
    if eng == "act":
        P.op("act", lambda h: h.copy(out, in_), reads, writes)
    else:
        P.op(eng, lambda h: h.tensor_copy(out, in_), reads, writes)


def memset(P, eng, ap, val, writes):
    P.op(eng, lambda h: h.memset(ap, val), (), writes)


def build_program():
    nc = bass.Bass("TRN2", target_bir_lowering=False)

    def din(name, shape):
        return nc.dram_tensor(name, list(shape), F32, kind="ExternalInput").ap()

    x_own = din("x_own", [TOWN, D])
    x_all = din("x_all", [2 * TOWN, D])
    x_halo = din("x_halo", [128, D])
    w_own = din("w_own", [D, 772])
    mem = din("mem", [256, D])
    flag_d = din("flag", [128, 1])
    w_in = din("w_in", [D, 4616])
    pw_w = din("conv_pw_w", [512, D])
    ab_w = din("attn_branch_w", [512, D])
    wout_w = din("w_out", [D, D])
    xq_w = din("xa_wq", [D, 512])
    xk_w = din("xa_wk", [D, 512])
    xv_w = din("xa_wv", [D, 512])
    xo_w = din("xa_wo", [512, D])
    eg_w = din("expert_w_gate", [16, D, 256])
    eu_w = din("expert_w_up", [16, D, 256])
    ed_w = din("expert_w_down", [16, 256, D])
    wr_d = din("wr", [D, 20])
    rb_d = din("rb_b", [128, 20])
    g_mix_d = din("g_mix_b", [128, D])
    g_xa_d = din("g_xa_b", [128, D])
    g_mem_d = din("g_mem_b", [128, D])
    g_moe_d = din("g_moe_b", [128, D])
    g_fin_d = din("g_fin_b", [128, D])
    bf_d = din("bf_b", [128, 4])
    dww_d = din("dw_w_t", [128, 4 * 31])
    dwb_d = din("dw_b_t", [128, 4])
    lng_d = din("ln_g_t", [128, 4])
    lnb_d = din("ln_b_t", [128, 4])
    ident_d = din("ident", [128, 128])
    uneg_d = din("uneg", [128, 128])
    e127_d = din("e127", [128, 128])
    mask_d = din("mask01", [128, 128])
    out_d = nc.dram_tensor("out", [TOWN, D], F32, kind="ExternalOutput").ap()
    ag_in = nc.dram_tensor("ag_in", [256, 2 * TOWN], BF16).ap()
    ag_out = nc.dram_tensor("ag_out", [512, 2 * TOWN], BF16).ap()
    if DEBUG:
        x1_d = nc.dram_tensor("x1_scr", [TOWN, D], F32, kind="ExternalOutput").ap()
    else:
        x1_d = nc.dram_tensor("x1_scr", [TOWN, D], F32).ap()

    dbg_outs = {}

    def dump(name, ap, shape):
        if not DEBUG:
            return
        t = nc.dram_tensor("dbg_" + name, list(shape), F32, kind="ExternalOutput").ap()
        dbg_outs[name] = t
        P.dma("pool", t, ap, "dbg_" + name)

    P = Prog(nc)
    P.banks = [P.ps("bk%d" % i, [128, 512], F32) for i in range(8)]
    ALLB = list(range(8))

    attnT = P.sb("attnT", [128, 4, TOWN], BF16, outer=True)
    idb = P.sb("idb", [128, 128], BF16, outer=True)
    ones_bf = P.sb("ones_bf", [128, 128], BF16, outer=True)
    ones_f = P.sb("ones_f", [128, 128], F32, outer=True)
    xs = [P.sb("xs%d" % i, [128, D], F32, outer=True) for i in range(2)]
    junk = P.sb("junk", [128, D], BF16, outer=True)
    hb = P.sb("hb", [128, D], BF16, outer=True)
    hb2 = P.sb("hb2", [128, D], BF16, outer=True)
    hb3 = P.sb("hb3", [128, D], BF16, outer=True)
    WcG = P.sb("WcG", [128, 8, 1024], BF16, outer=True)
    KxT = P.sb("KxT", [128, 4, 256], BF16, outer=True)
    Vx = P.sb("Vx", [128, 2, 512], BF16, outer=True)
    st = P.sb("st", [128, 12], F32, outer=True)

    hbs = [hb, hb2, hb3]
    POOL_NORM = [False]
    NDEP = 2

    def emit_stats(xs_ap, xs_key, gb, gb_key, k):
        o = 4 * k
        act(P, junk[:], xs_ap, AF.Square, [xs_key], ["junk", ("st0", k)], accum_out=st[:, o:o + 1])
        act(P, st[:, o + 2:o + 3], st[:, o:o + 1], AF.Ln, [("st0", k)], [("st2", k)], bias=EPS, scale=1.0 / D)
        act(P, st[:, o + 3:o + 4], st[:, o + 2:o + 3], AF.Exp, [("st2", k)], [("st3", k)], scale=-0.5)
        stt(P, "pool" if (POOL_NORM[0] and k == 1) else "dve", hbs[k][:], xs_ap, st[:, o + 3:o + 4], gb[:],
            ALU.mult, ALU.mult, [xs_key, ("st3", k), gb_key], [("hb", k)])

    def emit_T(k, hT, hT_key, col0, evac_eng):
        bk, bkey = P.bank("any", ALLB)
        tpb = bk[:].bitcast(BF16)
        for c in range(8):
            tr(P, tpb[:, c * 128:(c + 1) * 128], hbs[k][:, c * 128:(c + 1) * 128], idb[:], [("hb", k), "idb"], [bkey])
        cp(P, evac_eng, hT[:, :, col0:col0 + 128], tpb[:, 0:1024].rearrange("p (c n) -> p c n", c=8), [bkey], [hT_key])

    ncnt = [0]

    def emit_norm(xs_ap, xs_key, gb, gb_key, hT, hT_key, col0, evac_eng):
        k = ncnt[0] % 3
        ncnt[0] += 1
        emit_stats(xs_ap, xs_key, gb, gb_key, k)
        emit_T(k, hT, hT_key, col0, evac_eng)

    def emit_norm_seq(items, gb, gb_key, pre=None):
        n = len(items)
        ks = []
        LAG = 4
        for idx in range(n + NDEP + LAG):
            if idx < n:
                it = items[idx]
                if it[6] is not None:
                    it[6]()
                k = ncnt[0] % 3
                ncnt[0] += 1
                ks.append(k)
                emit_stats(it[0], it[1], gb, gb_key, k)
            if NDEP <= idx < n + NDEP:
                it = items[idx - NDEP]
                emit_T(ks[idx - NDEP], it[2], it[3], it[4], it[5])
            if pre is not None and idx >= NDEP + LAG:
                pre(idx - NDEP - LAG)

    sel_d = din("sel", [128, 4 * 128])
    NB = 32
    NH = 4
    P.push()
    KT = P.sb("KT", [128, 2, 4096], BF16)
    QTz = P.sb("QTz", [128, NH, 4096], BF16)
    Vp = P.sb("Vp", [128, NB, 256], BF16)
    Qaug = P.sb("Qaug", [128, 4096], BF16)
    Sel = P.sb("Sel", [128, NH, 128], BF16)
    zt = P.sb("zt", [128, NB, NH], F32)
    flag = P.sb("flag", [128, 2], F32)
    maskb = P.sb("maskb", [128, 128], BF16)
    attnO = P.sb("attnO", [128, 2, 4096], BF16)

    P.phase_begin()
    Wq = P.sb("Wqkvf", [128, 8, 772], BF16)
    hT = [P.sb("hT%d" % i, [128, 8, 512], BF16) for i in range(2)]
    gmix = P.sb("gmix", [128, D], F32)
    bfb = P.sb("bfb", [128, NH], F32)

    P.dma("pool", idb[:], ident_d, "c_idb", writes=["idb"])
    P.dma("sp", gmix[:], g_mix_d, "c_gmix", writes=["gmix"])
    P.dma("sp", bfb[:], bf_d, "c_bfb", writes=["bfb"])
    P.dma("pool", Wq[:, :, 512:772], w_own[:, 512:772].rearrange("(c p) n -> p c n", p=128), "w_qv", writes=["WqV"])
    P.dma("pool", Wq[:, :, 256:512], w_own[:, 256:512].rearrange("(c p) n -> p c n", p=128), "w_qk", writes=["WqK"])
    P.dma("pool", Wq[:, :, 0:256], w_own[:, 0:256].rearrange("(c p) n -> p c n", p=128), "w_qq", writes=["WqQ"])
    P.dma("pool", maskb[:], mask_d, "c_mask", writes=["maskb"])
    P.dma("pool", Sel[:].rearrange("p a b -> p (a b)"), sel_d, "c_sel", writes=["Sel"])
    P.dma("sp", flag[:, 0:1], flag_d, "c_flag", writes=["flag"])
    memset(P, "pool", ones_bf[:], 1.0, ["ones_bf"])
    memset(P, "pool", ones_f[:], 1.0, ["ones_f"])
    memset(P, "pool", QTz[:].rearrange("p a b -> p (a b)"), 0.0, ["QTz0"])
    memset(P, "pool", Qaug[:], 0.0, ["Qaug0"])

    def a_load(blk):
        P.dma("sp", xs[blk % 2][:], x_all[blk * 128:(blk + 1) * 128, :], "xs%d" % (blk % 2), writes=[("xs", blk % 2)])

    ev = [0]

    def a_post(blk):
        ch, t = blk // 4, blk % 4
        hTc = hT[ch % 2]
        hkey = ("hT", ch % 2)
        vb, vkey = P.bank("any", ALLB)
        fb, fkey = P.bank("any", ALLB)
        for c in range(8):
            mm(P, vb[:, 0:256], hTc[:, c, t * 128:(t + 1) * 128], Wq[:, c, 512:768], c == 0, c == 7,
               [hkey, "WqV"], [vkey])
        for c in range(8):
            mm(P, fb[:, 0:NH], hTc[:, c, t * 128:(t + 1) * 128], Wq[:, c, 768:772], c == 0, c == 7,
               [hkey, "WqV"], [fkey])
        cp(P, "act", Vp[:, blk, :], vb[:, 0:256], [vkey], [("Vp", blk)])
        tt(P, "dve", zt[:, blk, :], fb[:, 0:NH], bfb[:], ALU.add, [fkey, "bfb"], [("zt", blk)])
        if t != 3:
            return
        for p in range(2):
            kb, kkey = P.bank("any", ALLB)
            for c in range(8):
                mm(P, kb[:, :], Wq[:, c, 256 + p * 128:256 + (p + 1) * 128], hTc[:, c, :], c == 0, c == 7,
                   [hkey, "WqK"], [kkey])
            cp(P, "act" if ev[0] % 2 else "dve", KT[:, p, ch * 512:(ch + 1) * 512], kb[:, :], [kkey], [("KT", p, ch)])
            ev[0] += 1
        for p in range(2):
            qb, qkey = P.bank("any", ALLB)
            for c in range(8):
                mm(P, qb[:, :], Wq[:, c, p * 128:(p + 1) * 128], hTc[:, c, :], c == 0, c == 7,
                   [hkey, "WqQ"], [qkey])
            P.op("act", lambda h, o=QTz[0:64, 2 * p, ch * 512:(ch + 1) * 512], i=qb[0:64, :]: h.mul(o, i, 0.125),
                 [qkey, "QTz0"], [("QTz", 2 * p, ch)])
            ts(P, "dve", QTz[64:128, 2 * p + 1, ch * 512:(ch + 1) * 512], qb[64:128, :], 0.125, None, ALU.mult, None,
               [qkey, "QTz0"], [("QTz", 2 * p + 1, ch)])

    items = []
    for blk in range(NB):
        ch, t = blk // 4, blk % 4
        items.append((xs[blk % 2][:], ("xs", blk % 2), hT[ch % 2], ("hT", ch % 2), t * 128,
                      "act" if blk % 2 else "dve", (lambda blk=blk: a_load(blk))))
    emit_norm_seq(items, gmix, "gmix", pre=a_post)
    P.phase_end()

    P.phase_begin()
    NZ = NB * NH
    scA = P.sb("scA", [128, NB, NH], F32)
    scB = P.sb("scB", [128, NB, NH], F32)
    excl = P.sb("excl", [128, NB, NH], F32)
    ctok = P.sb("ctok", [128, NB, NH], F32)
    cendx = P.sb("cendx", [128, NB + 1, NH], F32)
    biasT = P.sb("biasT", [128, 8, NB, NH], F32)
    lq = P.sb("lq", [128, NB, NH], F32)
    lqhi = P.sb("lqhi", [128, NB, NH], BF16)
    lqlo = P.sb("lqlo", [128, NB, NH], BF16)
    Lst = P.sb("Lst", [128, NB, NH, 2], BF16)
    uneg = P.sb("uneg", [128, 128], F32)
    oneg = P.sb("oneg", [128, 128], F32)
    e127 = P.sb("e127", [128, 128], F32)
    Pt = [P.sb("Pt%d" % i, [128, 512], BF16) for i in range(4)]
    Et = [P.sb("Et%d" % i, [128, 512], F32) for i in range(2)]
    Tm = [P.sb("Tm%d" % i, [128, 512], F32) for i in range(2)]
    rd = P.sb("rd", [128, 512], F32)
    Vh = [P.sb("Vh%d" % i, [128, NB, 128], BF16) for i in range(2)]

    P.dma("sp", uneg[:], uneg_d, "c_uneg", writes=["uneg"])
    P.dma("sp", e127[:], e127_d, "c_e127", writes=["e127"])
    P.dma("pool", WcG[:], w_in[:, 0:1024].rearrange("(c p) n -> p c n", p=128), "w_cg")
    memset(P, "pool", oneg[:], -1.0, ["oneg"])
    memset(P, "pool", excl[:, 0, :], 0.0, ["excl0"])
    memset(P, "pool", cendx[:, 0, :], 0.0, ["cendx0"])
    for b_ in range(2):
        memset(P, "pool", Vh[b_][:, :, 64:128], 1.0, [("Vh1", b_)])

    fl = lambda t_: t_[:].rearrange("p a b -> p (a b)")
    ztf = fl(zt)
    act(P, fl(scA), ztf, AF.Exp, [], ["scA"], scale=-1.0)
    act(P, ztf, fl(scA), AF.Ln, ["scA"], ["nl"], bias=1.0)
    srcT, dstT = zt, scA
    skey, dkey = "nl", "scA"
    for s_ in (1, 2, 4, 8, 16):
        tt(P, "dve", dstT[:, s_:, :], srcT[:, s_:, :], srcT[:, :NB - s_, :], ALU.add, [skey], [dkey])
        cp(P, "dve", dstT[:, :s_, :], srcT[:, :s_, :], [skey], [dkey])
        if s_ == 1:
            srcT, dstT = scA, scB
            skey, dkey = "scA", "scB"
        else:
            srcT, dstT = dstT, srcT
            skey, dkey = dkey, skey
    cp(P, "dve", excl[:, 1:, :], srcT[:, :NB - 1, :], [skey], ["excl"])
    cb, ckey = P.bank("any", ALLB)
    mm(P, cb[:, 0:NZ], uneg[:], ztf, True, False, ["uneg", "nl"], [ckey])
    mm(P, cb[:, 0:NZ], oneg[:], fl(excl), False, True, ["oneg", "excl", "excl0"], [ckey])
    cp(P, "dve", fl(ctok), cb[:, 0:NZ], [ckey], ["ctok"])
    eb, ekey = P.bank("any", ALLB)
    mm(P, eb[:, 0:NZ], e127[:], fl(ctok), True, True, ["e127", "ctok"], [ekey])
    cp(P, "dve", cendx[:, 1:, :].rearrange("p a b -> p (a b)"), eb[:, 0:NZ], [ekey, "cendx0"], ["cendx"])
    cref = cendx[:, 0:NB:4, :]
    tt(P, "dve", biasT[:], cref.unsqueeze(2).to_broadcast([128, 8, NB, NH]),
       ctok[:].unsqueeze(1).to_broadcast([128, 8, NB, NH]), ALU.subtract, ["ctok", "cendx"], ["biasT"])
    tt(P, "dve", lq[:].rearrange("p (i t) h -> p i t h", i=8), ctok[:].rearrange("p (i t) h -> p i t h", i=8),
       cref.unsqueeze(2).to_broadcast([128, 8, 4, NH]), ALU.subtract, ["ctok", "cendx"], ["lq"])
    cp(P, "dve", lqhi[:], lq[:], ["lq"], ["lqhi"])
    tt(P, "dve", lqlo[:], lq[:], lqhi[:], ALU.subtract, ["lq", "lqhi"], ["lqlo"])
    cp(P, "dve", Lst[:, :, :, 0], lqhi[:], ["lqhi"], ["Lst"])
    cp(P, "dve", Lst[:, :, :, 1], lqlo[:], ["lqlo"], ["Lst"])
    for i in range(8):
        tb, tkey = P.bank("any", ALLB)
        tpb = tb[:].bitcast(BF16)
        for t in range(4):
            tr(P, tpb[0:2 * NH, t * 128:(t + 1) * 128], Lst[:, 4 * i + t].rearrange("p a b -> p (a b)"), idb[:],
               ["Lst"], [tkey])
        cp(P, "dve", Qaug[0:2 * NH, i * 512:(i + 1) * 512], tpb[0:2 * NH, 0:512], [tkey, "Qaug0"], [("Qaug", i)])

    SB_IDS = [0, 1, 2, 3]
    O_IDS = [4, 5, 6, 7]
    NPT = 4
    SKEW = 2
    pcount = 0
    ecount = 0
    for h_ in range(NH):
        p = h_ // 2
        r0 = (h_ % 2) * 64
        vh = Vh[h_ % 2]
        vhk = ("Vh", h_ % 2)
        if h_ == 0:
            cp(P, "dve", vh[:, :, 0:64], Vp[:, :, 0:64], [], [vhk])
        for i in range(8):
            if i == 2 and h_ + 1 < NH:
                cp(P, "dve", Vh[(h_ + 1) % 2][:, :, 0:64], Vp[:, :, (h_ + 1) * 64:(h_ + 2) * 64], [],
                   [("Vh", (h_ + 1) % 2)])
            noff = 4 * i
            nj = noff + 4
            qcols = slice(i * 512, (i + 1) * 512)
            odg, odgk = P.bank("O", O_IDS)
            if noff > 0:
                ooff, ooffk = P.bank("O", O_IDS)
                ebk, ebkey = P.bank("S", SB_IDS)
                mm(P, ebk[:, :], Sel[:, h_, :], Qaug[:, qcols], True, True, [("Qaug", i)], [ebkey])
                et = Et[ecount % 2]
                etk = ("Et", ecount % 2)
                act(P, et[:], ebk[:, :], AF.Exp, [ebkey], [etk])
            ecount += 1
            pend = []

            def emit_pv(j, q0, ptile, pkey):
                if j < noff:
                    mm(P, ooff[:, :], vh[:, j, :], ptile[:, :], j == 0, j == noff - 1,
                       [vhk, ("Vh1", h_ % 2), pkey], [ooffk])
                else:
                    mm(P, odg[:, q0:512], vh[:, j, :], ptile[:, q0:512], j == noff, j == nj - 1,
                       [vhk, ("Vh1", h_ % 2), pkey], [odgk])

            for j in range(nj):
                r = j - noff
                q0 = r * 128 if r > 0 else 0
                sbk, skey_ = P.bank("S", SB_IDS)
                diag = r >= 0
                mm(P, sbk[:, q0:512], KT[:, p, j * 128:(j + 1) * 128],
                   QTz[:, h_, i * 512 + q0:(i + 1) * 512], True, not diag, [], [skey_])
                if diag:
                    mm(P, sbk[:, q0:512], Sel[:, h_, :], Qaug[:, i * 512 + q0:(i + 1) * 512], False, True,
                       [("Qaug", i)], [skey_])
                if len(pend) >= SKEW:
                    emit_pv(*pend.pop(0))
                ptile = Pt[pcount % NPT]
                pkey = ("Pt", pcount % NPT)
                pcount += 1
                act(P, ptile[:, q0:512], sbk[:, q0:512], AF.Exp, [skey_, "biasT"], [pkey],
                    bias=biasT[:, i, j, h_:h_ + 1])
                if diag:
                    tt(P, "pool", ptile[:, q0:q0 + 128], ptile[:, q0:q0 + 128], maskb[:], ALU.mult,
                       [pkey], [pkey])
                pend.append((j, q0, ptile, pkey))
            while pend:
                emit_pv(*pend.pop(0))
            tm = Tm[ecount % 2]
            tmk = ("Tm", ecount % 2)
            if noff > 0:
                tt(P, "dve", tm[:], ooff[:, :], et[:], ALU.mult, [ooffk, etk], [tmk])
                tt(P, "dve", tm[:], tm[:], odg[:, :], ALU.add, [tmk, odgk], [tmk])
            else:
                cp(P, "dve", tm[:], odg[:, :], [odgk], [tmk])
            P.op("dve", lambda h, o=rd[0:64, :], i_=tm[64:128, :]: h.reciprocal(o, i_), [tmk], ["rd"])
            tt(P, "dve", attnO[r0:r0 + 64, p, qcols], tm[0:64, :], rd[0:64, :], ALU.mult,
               [tmk, "rd"], [("attnO", p)])
        if h_ % 2 == 1:
            P.dma("sp", ag_in[p * 128:(p + 1) * 128, :], attnO[:, p, :], "ag_w%d" % p, reads=[("attnO", p)],
                  writes=[("ag_in", p)])
    P.phase_end()

    P.phase_begin()
    tA = P.sb("tA", [128, 4, TOWN], BF16)
    tB = P.sb("tB", [128, 4, TOWN], BF16)
    P.collective("AllGather", [ag_in.opt()], [ag_out.opt()], [[0, 1], [2, 3], [4, 5], [6, 7]], [], ["ag_out"])
    Wxk = P.sb("Wxk", [128, 8, 512], BF16)
    Wxv = P.sb("Wxv", [128, 8, 512], BF16)
    gmem = P.sb("gmem", [128, D], F32)
    hmT = P.sb("hmT", [128, 8, 256], BF16)
    P.dma("pool", Wxk[:], xk_w.rearrange("(c p) n -> p c n", p=128), "w_xk", writes=["Wxk"])
    P.dma("pool", Wxv[:], xv_w.rearrange("(c p) n -> p c n", p=128), "w_xv", writes=["Wxv"])
    P.dma("sp", gmem[:], g_mem_d, "c_gmem", writes=["gmem"])
    for mb in range(2):
        P.dma("sp", xs[mb][:], mem[mb * 128:(mb + 1) * 128, :], "xs%d" % mb, writes=[("xs", mb)])
        emit_norm(xs[mb][:], ("xs", mb), gmem, "gmem", hmT, "hmT", mb * 128, "dve")
    for h_ in range(4):
        kb, kkey = P.bank("any", ALLB)
        for c in range(8):
            mm(P, kb[:, 0:256], Wxk[:, c, h_ * 128:(h_ + 1) * 128], hmT[:, c, :], c == 0, c == 7, ["Wxk", "hmT"], [kkey])
        cp(P, "dve", KxT[:, h_, :], kb[:, 0:256], [kkey], ["KxT"])
    for mb in range(2):
        vb, vkey = P.bank("any", ALLB)
        for c in range(8):
            mm(P, vb[:, :], hmT[:, c, mb * 128:(mb + 1) * 128], Wxv[:, c, :], c == 0, c == 7, ["Wxv", "hmT"], [vkey])
        cp(P, "act", Vx[:, mb, :], vb[:, :], [vkey], ["Vx"])
    P.dma("sp", tA[:], ag_out[:, 0:TOWN].rearrange("(a q) n -> q a n", q=128), "ag_rA", reads=["ag_out"], writes=["tA"])
    P.dma("sp", tB[:], ag_out[:, TOWN:2 * TOWN].rearrange("(a q) n -> q a n", q=128), "ag_rB", reads=["ag_out"], writes=["tB"])
    ts(P, "dve", flag[:, 1:2], flag[:, 0:1], -1.0, 1.0, ALU.mult, ALU.add, [], ["fl2"])
    fl2 = lambda t_: t_[:].rearrange("p a b -> p (a b)")
    ts(P, "dve", fl2(tA), fl2(tA), flag[:, 1:2], None, ALU.mult, None, ["tA", "fl2"], ["tA"])
    stt(P, "dve", fl2(attnT), fl2(tB), flag[:, 0:1], fl2(tA), ALU.mult, ALU.add, ["tA", "tB"], ["attnT"])
    if DEBUG:
        P.barrier()
        dump("attnT", attnT[:].rearrange("p a b -> p (a b)"), [128, 4 * TOWN])
    P.phase_end()
    P.pop()

    P.phase_begin()
    Wc = P.sb("Wc", [128, 8, 2048], BF16)
    Wpw = P.sb("Wpw", [128, 4, D], BF16)
    Wab = P.sb("Wab", [128, 4, D], BF16)
    Wo = P.sb("Wout", [128, 8, D], BF16)
    gmix2 = P.sb("gmix2", [128, D], F32)
    dww = P.sb("dww", [128, 4, 31], F32)
    dwb = P.sb("dwb", [128, 4], F32)
    lng = P.sb("lng", [128, 4], F32)
    lnb = P.sb("lnb", [128, 4], F32)
    hTa = P.sb("hTa", [128, 8, 512], BF16)
    uT = P.sb("uT", [128, 4, 544], BF16)
    Dg = P.sb("Dg", [128, 31, 128], BF16)
    lnA = [P.sb("lnA%d" % i, [128, 512], BF16) for i in range(2)]
    lnB = [P.sb("lnB%d" % i, [128, 512], BF16) for i in range(2)]
    mgA = [P.sb("mgA%d" % i, [128, 512], BF16) for i in range(2)]
    cvb = P.sb("cvb", [128, 4, 512], BF16)
    sqb = P.sb("sqb", [128, 4, 512], BF16)
    su = P.sb("su", [128, 4, 512], BF16)
    sgc = P.sb("sgc", [128, 8, 512], BF16)
    sga = P.sb("sga", [128, 8, 512], BF16)
    mg = P.sb("mg", [128, 8, 512], BF16)
    t1 = P.sb("t1", [128, 512], F32)
    t2 = P.sb("t2", [128, 512], F32)
    t3 = P.sb("t3", [128, 512], F32)
    t4 = P.sb("t4", [128, 512], F32)
    mean_s = P.sb("mean_s", [128, 512], F32)
    rstd_s = P.sb("rstd_s", [128, 512], F32)
    oneS = P.sb("oneS", [128, 128], BF16)
    xr = [P.sb("xr%d" % i, [128, D], F32) for i in range(2)]

    P.dma("pool", Wc[:, :, 0:1024], w_in[:, 2568:3592].rearrange("(c p) n -> p c n", p=128), "w_c", writes=["Wc"])
    P.dma("pool", Wc[:, :, 1024:2048], w_in[:, 3592:4616].rearrange("(c p) n -> p c n", p=128), "w_c", writes=["Wc"])
    P.dma("pool", Wpw[:], pw_w.rearrange("(c p) n -> p c n", p=128), "w_pw", writes=["Wpw"])
    P.dma("pool", Wab[:], ab_w.rearrange("(c p) n -> p c n", p=128), "w_ab", writes=["Wab"])
    P.dma("pool", Wo[:], wout_w.rearrange("(c p) n -> p c n", p=128), "w_out", writes=["Wout"])
    P.dma("sp", gmix2[:], g_mix_d, "c_gmix2", writes=["gmix2"])
    P.dma("sp", dww[:].rearrange("p a b -> p (a b)"), dww_d, "c_dww", writes=["dww"])
    P.dma("sp", dwb[:], dwb_d, "c_dwb", writes=["dwb"])
    P.dma("sp", lng[:], lng_d, "c_lng", writes=["lng"])
    P.dma("sp", lnb[:], lnb_d, "c_lnb", writes=["lnb"])
    memset(P, "pool", oneS[:], 1.0 / 512, ["oneS"])

    def glu_tiles(ncol, dst_col0):
        for m in range(4):
            ab_, akey = P.bank("any", ALLB)
            gb_, gkey = P.bank("any", ALLB)
            for c in range(8):
                mm(P, ab_[:, 0:ncol], WcG[:, c, m * 128:(m + 1) * 128], hTa[:, c, 0:ncol], c == 0, c == 7,
                   ["hTa"], [akey])
            for c in range(8):
                mm(P, gb_[:, 0:ncol], WcG[:, c, 512 + m * 128:512 + (m + 1) * 128], hTa[:, c, 0:ncol], c == 0, c == 7,
                   ["hTa"], [gkey])
            tg = (t1, t2)[m % 2]
            tgk = ("tg", m % 2)
            act(P, tg[:, 0:ncol], gb_[:, 0:ncol], AF.Sigmoid, [gkey], [tgk])
            tt(P, "dve", uT[:, m, dst_col0:dst_col0 + ncol], ab_[:, 0:ncol], tg[:, 0:ncol], ALU.mult,
               [akey, tgk], [("uT", m)])

    P.dma("sp", xs[0][:], x_halo, "xs0", writes=[("xs", 0)])
    emit_norm(xs[0][:], ("xs", 0), gmix2, "gmix2", hTa, "hTa", 0, "dve")
    for m in range(4):
        memset(P, "pool", uT[:, m, 0:32], 0.0, [("uT", m)])
    glu_tiles(128, 32)
    for m in range(4):
        cp(P, "dve", uT[:, m, 2:32], uT[:, m, 130:160], [("uT", m)], [("uT", m)])

    def emit_gates(i, f):
        gcb, gckey = P.bank("any", ALLB)
        gab, gakey = P.bank("any", ALLB)
        for c in range(8):
            mm(P, gcb[:, :], Wc[:, c, f * 128:(f + 1) * 128], hTa[:, c, :], c == 0, c == 7,
               ["hTa", "Wc"], [gckey])
        for c in range(8):
            mm(P, gab[:, :], Wc[:, c, 1024 + f * 128:1024 + (f + 1) * 128], hTa[:, c, :], c == 0, c == 7,
               ["hTa", "Wc"], [gakey])
        act(P, sgc[:, f, :], gcb[:, :], AF.Sigmoid, [gckey], [("sgc", f)])
        act(P, sga[:, f, :], gab[:, :], AF.Sigmoid, [gakey], [("sga", f)])

    def ca_front(i):
        items = []
        for t in range(4):
            blk = i * 4 + t
            k2 = blk % 2

            def ld(blk=blk, k2=k2):
                P.dma("sp", xs[k2][:], x_own[blk * 128:(blk + 1) * 128, :], "xs%d" % k2, writes=[("xs", k2)])

            items.append((xs[k2][:], ("xs", k2), hTa, "hTa", t * 128, "act" if t % 2 else "dve", ld))
        emit_norm_seq(items, gmix2, "gmix2")

    ca_front(0)
    for i in range(4):
        def gen_dg(m):
            tt(P, "dve", Dg[:], idb[:].unsqueeze(1).to_broadcast([128, 31, 128]),
               dww[:, m, :].unsqueeze(2).to_broadcast([128, 31, 128]), ALU.mult, ["dww"], ["Dg"])

        gen_dg(0)
        glu_tiles(512, 32)
        for m in range(4):
            if m > 0:
                gen_dg(m)
            cb_, ckey_ = P.bank("any", ALLB)
            for w in range(31):
                mm(P, cb_[:, :], Dg[:, w, :], uT[:, m, 2 + w:2 + w + 512], w == 0, w == 30, ["Dg", ("uT", m)], [ckey_])
            act(P, cvb[:, m, :], cb_[:, :], AF.Identity, [ckey_, "dwb"], [("cvb", m)], bias=dwb[:, m:m + 1])
            act(P, sqb[:, m, :], cb_[:, :], AF.Square, [ckey_, "dwb"], [("sqb", m)], bias=dwb[:, m:m + 1])
            cp(P, "pool", uT[:, m, 2:32], uT[:, m, 514:544], [("uT", m)], [("uT", m)])
            emit_gates(i, 2 * m)
            emit_gates(i, 2 * m + 1)
        mb_, mkey = P.bank("any", ALLB)
        qb_, qkey_ = P.bank("any", ALLB)
        for m in range(4):
            mm(P, mb_[:, :], oneS[:], cvb[:, m, :], m == 0, m == 3, ["oneS", ("cvb", m)], [mkey])
        for m in range(4):
            mm(P, qb_[:, :], oneS[:], sqb[:, m, :], m == 0, m == 3, ["oneS", ("sqb", m)], [qkey_])
        ln_ops = []
        ln_ops.append(lambda: cp(P, "act", mean_s[:], mb_[:, :], [mkey], ["mean_s"]))
        ln_ops.append(lambda: tt(P, "dve", t2[:], mean_s[:], mean_s[:], ALU.mult, ["mean_s"], ["t2"]))
        ln_ops.append(lambda: tt(P, "dve", t3[:], qb_[:, :], t2[:], ALU.subtract, [qkey_, "t2"], ["t3"]))
        ln_ops.append(lambda: act(P, t2[:], t3[:], AF.Ln, ["t3"], ["t2"], bias=EPS))
        ln_ops.append(lambda: act(P, rstd_s[:], t2[:], AF.Exp, ["t2"], ["rstd_s"], scale=-0.5))
        ln_ops.append(lambda: stt(P, "dve", t4[:], mean_s[:], -1.0, rstd_s[:], ALU.mult, ALU.mult,
                                  ["mean_s", "rstd_s"], ["nb"]))

        def ln_m(m):
            ta = lnA[m % 2]
            tb_ = lnB[m % 2]
            tt(P, "dve", ta[:], cvb[:, m, :], rstd_s[:], ALU.mult, [("cvb", m), "rstd_s"], [("lnA", m % 2)])
            tt(P, "dve", tb_[:], ta[:], t4[:], ALU.add, [("lnA", m % 2), "nb"], [("lnB", m % 2)])
            act(P, su[:, m, :], tb_[:], AF.Silu, [("lnB", m % 2), "lng", "lnb"], [("su", m)], bias=lnb[:, m:m + 1],
                scale=lng[:, m:m + 1])

        for m in range(4):
            ln_ops.append(lambda m=m: ln_m(m))

        def ab_f(f):
            aob, aokey = P.bank("any", ALLB)
            for c in range(4):
                mm(P, aob[:, :], Wab[:, c, f * 128:(f + 1) * 128], attnT[:, c, i * 512:(i + 1) * 512], c == 0, c == 3,
                   ["Wab"], [aokey])
            tt(P, "dve", mg[:, f, :], aob[:, :], sga[:, f, :], ALU.mult, [aokey, ("sga", f)], [("mg", f)])

        ab_ops = [(lambda f=f: ab_f(f)) for f in range(8)]
        while ln_ops or ab_ops:
            if ab_ops:
                ab_ops.pop(0)()
            if ln_ops:
                ln_ops.pop(0)()
        for f in range(8):
            cob, cokey = P.bank("any", ALLB)
            for c in range(4):
                mm(P, cob[:, :], Wpw[:, c, f * 128:(f + 1) * 128], su[:, c, :], c == 0, c == 3,
                   ["Wpw", ("su", c)], [cokey])
            ma = mgA[f % 2]
            tt(P, "dve", ma[:], cob[:, :], sgc[:, f, :], ALU.mult, [cokey, ("sgc", f)], [("mgA", f % 2)])
            tt(P, "dve", mg[:, f, :], mg[:, f, :], ma[:], ALU.add, [("mgA", f % 2), ("mg", f)], [("mg", f)])
        if i + 1 < 4:
            ca_front(i + 1)
        for t in range(4):
            blk = i * 4 + t
            xb = xr[t % 2]
            xk = ("xr", t % 2)
            P.dma("sp", xb[:], x_own[blk * 128:(blk + 1) * 128, :], "xr%d" % (t % 2), writes=[xk])
            for n in range(2):
                db, dkey_ = P.bank("any", ALLB)
                for c in range(8):
                    mm(P, db[:, :], mg[:, c, t * 128:(t + 1) * 128], Wo[:, c, n * 512:(n + 1) * 512], c == 0, c == 7,
                       [("mg", c), "Wout"], [dkey_])
                tt(P, "dve", xb[:, n * 512:(n + 1) * 512], xb[:, n * 512:(n + 1) * 512], db[:, :], ALU.add,
                   [xk, dkey_], [xk])
            P.dma("sp", x1_d[blk * 128:(blk + 1) * 128, :], xb[:], "x1w%d" % (t % 2), reads=[xk], writes=[("x1", blk)])
    P.phase_end()

    P.push()
    xacc = P.sb("xacc", [128, 16, D], F32)
    h2T = P.sb("h2T", [128, 8, TOWN], BF16)
    comb = P.sb("comb", [128, 16, 16], F32)
    sa = [P.sb("sa%d" % i, [128, 512], BF16) for i in range(2)]
    uu = [P.sb("uu%d" % i, [128, 512], BF16) for i in range(4)]

    def emit_expert_sub(e, s, wgf, wdf, wkeys):
        hk = ("h2T", s)
        for m in range(2):
            ab_, akey = P.bank("any", ALLB)
            bb_, bkey = P.bank("any", ALLB)
            for c in range(8):
                mm(P, ab_[:, :], wgf(c, m * 128, (m + 1) * 128), h2T[:, c, s * 512:(s + 1) * 512], c == 0, c == 7,
                   wkeys + [hk], [akey])
            for c in range(8):
                mm(P, bb_[:, :], wgf(c, 256 + m * 128, 256 + (m + 1) * 128), h2T[:, c, s * 512:(s + 1) * 512],
                   c == 0, c == 7, wkeys + [hk], [bkey])
            act(P, sa[m][:], ab_[:, :], AF.Silu, [akey], [("sa", m)])
            tt(P, "dve", uu[(s % 2) * 2 + m][:], bb_[:, :], sa[m][:], ALU.mult, [bkey, ("sa", m)],
               [("uu", (s % 2) * 2 + m)])
        for t in range(4):
            lb = s * 4 + t
            for n in range(2):
                yb, ykey = P.bank("any", ALLB)
                for m in range(2):
                    mm(P, yb[:, :], uu[(s % 2) * 2 + m][:, t * 128:(t + 1) * 128], wdf(m, n),
                       m == 0, m == 1, [("uu", (s % 2) * 2 + m)] + wkeys, [ykey])
                stt(P, "dve", xacc[:, lb, n * 512:(n + 1) * 512], yb[:, :], comb[:, lb, e:e + 1],
                    xacc[:, lb, n * 512:(n + 1) * 512], ALU.mult, ALU.add,
                    [ykey, ("comb", lb // 4), ("xacc", lb)], [("xacc", lb)])

    wgf0 = lambda c, lo, hi: WcG[:, c, lo:hi]
    wdf0 = lambda m, n: WcG[:, 2 * m + n, 512:1024]

    P.phase_begin()
    Wxq = P.sb("Wxq", [128, 8, 512], BF16)
    Wxo = P.sb("Wxo", [128, 4, D], BF16)
    gxa = P.sb("gxa", [128, D], F32)
    h1T = P.sb("h1T", [128, 8, 512], BF16)
    qxT = P.sb("qxT", [128, 4, 512], BF16)
    Px = [P.sb("Px%d" % i, [128, 512], BF16) for i in range(4)]
    oxTs = [P.sb("oxT%d" % i, [128, 4, 512], BF16) for i in range(2)]
    rdx = P.sb("rdx", [128, 512], F32)
    P.dma("pool", Wxq[:], xq_w.rearrange("(c p) n -> p c n", p=128), "w_xq", writes=["Wxq"])
    P.dma("pool", Wxo[:], xo_w.rearrange("(c p) n -> p c n", p=128), "w_xo", writes=["Wxo"])
    P.dma("sp", gxa[:], g_xa_d, "c_gxa", writes=["gxa"])
    P.dma("pool", WcG[:, :, 0:256], eg_w[0].rearrange("(c p) n -> p c n", p=128), "w_e0p", writes=["We0"])
    P.dma("pool", WcG[:, :, 256:512], eu_w[0].rearrange("(c p) n -> p c n", p=128), "w_e0p", writes=["We0"])
    for m_ in range(2):
        P.dma("pool", WcG[:, 2 * m_:2 * m_ + 2, 512:1024],
              ed_w[0][m_ * 128:(m_ + 1) * 128, :].rearrange("p (n f) -> p n f", n=2), "w_e0p", writes=["We0"])
    Wr = P.sb("Wr", [128, 8, 20], BF16)
    rbb = P.sb("rbb", [128, 20], F32)
    gmoe = P.sb("gmoe", [128, D], F32)
    rL = P.sb("rL", [128, 4, 20], F32)
    rS = P.sb("rS", [128, 6, 4], F32)
    rG = P.sb("rG", [128, 3, 4, 4], F32)
    rE = P.sb("rE", [128, 3, 4, 16], F32)
    P.dma("pool", Wr[:], wr_d.rearrange("(c p) n -> p c n", p=128), "w_r", writes=["Wr"])
    P.dma("sp", rbb[:], rb_d, "c_rbb", writes=["rbb"])
    P.dma("sp", gmoe[:], g_moe_d, "c_gmoe", writes=["gmoe"])
    BIG = 30000.0

    def emit_moe_front(lb0):
        emit_moe_norms(lb0)
        emit_moe_router(lb0)

    def emit_moe_norms(lb0):
        for lb in range(lb0, lb0 + 4):
            emit_norm(xacc[:, lb, :], ("xacc", lb), gmoe, "gmoe", h2T, ("h2T", lb // 4), lb * 128, "act" if lb % 2 else "dve")

    def emit_moe_router(lb0):
        rb_, rkey = P.bank("any", ALLB)
        for k in range(4):
            lb = lb0 + k
            for c in range(8):
                mm(P, rb_[:, k * 20:(k + 1) * 20], h2T[:, c, lb * 128:(lb + 1) * 128], Wr[:, c, :], c == 0, c == 7,
                   [("h2T", lb // 4), "Wr"], [rkey])
        AXX = mybir.AxisListType.X
        L = rL[:]
        tt(P, "dve", L, rb_[:, 0:80].rearrange("p (k n) -> p k n", k=4), rbb[:].unsqueeze(1).to_broadcast([128, 4, 20]),
           ALU.add, [rkey, "rbb"], ["rL"])
        P.op("dve", lambda h: h.tensor_reduce(rS[:, 0, :], rL[:, :, 0:4], AXX, ALU.max), ["rL"], ["gmax"])
        tt(P, "dve", rG[:, 0], rL[:, :, 0:4], rS[:, 0, :].unsqueeze(2).to_broadcast([128, 4, 4]), ALU.is_ge,
           ["rL", "gmax"], ["gmask"])
        tt(P, "dve", rG[:, 1], rL[:, :, 0:4], rS[:, 0, :].unsqueeze(2).to_broadcast([128, 4, 4]), ALU.subtract,
           ["rL", "gmax"], ["gsh"])
        act(P, rG[:, 1], rG[:, 1], AF.Exp, ["gsh"], ["geg"])
        P.op("dve", lambda h: h.tensor_reduce(rS[:, 1, :], rG[:, 1], AXX, ALU.add), ["geg"], ["gsum"])
        ts(P, "dve", rG[:, 2], rG[:, 0], BIG, -BIG, ALU.mult, ALU.add, ["gmask"], ["pen"])
        tt(P, "dve", rE[:, 0].rearrange("p k (g e) -> p k g e", g=4),
           rL[:, :, 4:20].rearrange("p k (g e) -> p k g e", g=4),
           rG[:, 2].unsqueeze(3).to_broadcast([128, 4, 4, 4]), ALU.add, ["rL", "pen"], ["lem"])
        P.op("dve", lambda h: h.tensor_reduce(rS[:, 2, :], rE[:, 0], AXX, ALU.max), ["lem"], ["m1"])
        tt(P, "dve", rE[:, 1], rE[:, 0], rS[:, 2, :].unsqueeze(2).to_broadcast([128, 4, 16]), ALU.is_ge,
           ["lem", "m1"], ["mask1"])
        stt(P, "dve", rE[:, 2], rE[:, 1], -BIG, rE[:, 0], ALU.mult, ALU.add, ["mask1", "lem"], ["lem2"])
        P.op("dve", lambda h: h.tensor_reduce(rS[:, 3, :], rE[:, 2], AXX, ALU.max), ["lem2"], ["m2"])
        tt(P, "dve", rE[:, 1], rE[:, 0], rS[:, 3, :].unsqueeze(2).to_broadcast([128, 4, 16]), ALU.is_ge,
           ["lem", "m2", "lem2"], ["selm"])
        tt(P, "dve", rE[:, 2], rE[:, 0], rS[:, 2, :].unsqueeze(2).to_broadcast([128, 4, 16]), ALU.subtract,
           ["lem", "m1", "selm"], ["esh"])
        act(P, rE[:, 2], rE[:, 2], AF.Exp, ["esh"], ["eex"])
        tt(P, "dve", rE[:, 2], rE[:, 2], rE[:, 1], ALU.mult, ["eex", "selm"], ["eex2"])
        P.op("dve", lambda h: h.tensor_reduce(rS[:, 4, :], rE[:, 2], AXX, ALU.add), ["eex2"], ["ssum"])
        tt(P, "dve", rS[:, 5, :], rS[:, 1, :], rS[:, 4, :], ALU.mult, ["gsum", "ssum"], ["coef0"])
        P.op("dve", lambda h: h.reciprocal(rS[:, 5, :], rS[:, 5, :]), ["coef0"], ["coef"])
        tt(P, "dve", comb[:, lb0:lb0 + 4, :], rE[:, 2], rS[:, 5, :].unsqueeze(2).to_broadcast([128, 4, 16]), ALU.mult,
           ["eex2", "coef"], [("comb", lb0 // 4)])


    XS = float(128 ** -0.5)
    pxc = [0]

    def cb_A(s):
        oxT = oxTs[s % 2]
        items = []
        for t in range(4):
            lb = s * 4 + t

            def ld(lb=lb):
                P.dma("sp", xacc[:, lb, :], x1_d[lb * 128:(lb + 1) * 128, :], "xacc%d" % lb, writes=[("xacc", lb)])

            items.append((xacc[:, lb, :], ("xacc", lb), h1T, "h1T", t * 128, "act" if t % 2 else "dve", ld))
        emit_norm_seq(items, gxa, "gxa")
        for h_ in range(4):
            qb, qkey = P.bank("any", ALLB)
            for c in range(8):
                mm(P, qb[:, :], Wxq[:, c, h_ * 128:(h_ + 1) * 128], h1T[:, c, :], c == 0, c == 7, ["Wxq", "h1T"], [qkey])
            P.op("act", lambda h, o=qxT[:, h_, :], i_=qb[:, :]: h.mul(o, i_, XS), [qkey], [("qxT", h_)])
        def x_s1(h_):
            pts = []
            for mb in range(2):
                sb_, skey_ = P.bank("any", ALLB)
                mm(P, sb_[:, :], KxT[:, h_, mb * 128:(mb + 1) * 128], qxT[:, h_, :], True, True,
                   [("qxT", h_)], [skey_])
                ptile = Px[pxc[0] % 4]
                pkey = ("Px", pxc[0] % 4)
                pxc[0] += 1
                act(P, ptile[:], sb_[:, :], AF.Exp, [skey_], [pkey])
                pts.append((ptile, pkey))
            return pts

        def x_s2(h_, pts):
            db, dkey_ = P.bank("any", ALLB)
            ob, okey = P.bank("any", ALLB)
            for mb in range(2):
                mm(P, db[:, :], ones_bf[:], pts[mb][0][:], mb == 0, mb == 1, [pts[mb][1]], [dkey_])
            for mb in range(2):
                mm(P, ob[:, :], Vx[:, mb, h_ * 128:(h_ + 1) * 128], pts[mb][0][:], mb == 0, mb == 1,
                   [pts[mb][1]], [okey])
            act(P, rdx[:], db[:, :], AF.Ln, [dkey_], ["rdx"])
            act(P, rdx[:], rdx[:], AF.Exp, ["rdx"], ["rdx"], scale=-1.0)
            tt(P, "dve", oxT[:, h_, :], ob[:, :], rdx[:], ALU.mult, [okey, "rdx"], [("oxT", s % 2, h_)])

        prev = None
        for h_ in range(4):
            cur = (h_, x_s1(h_))
            if prev is not None:
                x_s2(*prev)
            prev = cur
        x_s2(*prev)

    def cb_B(s, fill=None):
        oxT = oxTs[s % 2]
        for t in range(4):
            lb = s * 4 + t
            for n in range(2):
                wb_, wkey = P.bank("any", ALLB)
                for c in range(4):
                    mm(P, wb_[:, :], oxT[:, c, t * 128:(t + 1) * 128], Wxo[:, c, n * 512:(n + 1) * 512], c == 0, c == 3,
                       [("oxT", s % 2, c), "Wxo"], [wkey])
                tt(P, "dve", xacc[:, lb, n * 512:(n + 1) * 512], xacc[:, lb, n * 512:(n + 1) * 512], wb_[:, :], ALU.add,
                   [("xacc", lb), wkey], [("xacc", lb)])
        if fill is None:
            emit_moe_front(s * 4)
        else:
            fill(0)
            emit_moe_norms(s * 4)
            fill(1)
            emit_moe_router(s * 4)
            fill(2)

    cb_A(0)
    for s in range(4):
        if s + 1 < 4:
            cb_A(s + 1)
        if s == 3:
            cb_B(s, fill=lambda s0: emit_expert_sub(0, s0, wgf0, wdf0, ["We0"]))
        else:
            cb_B(s)
    POOL_NORM[0] = False
    P.phase_end()

    P.phase_begin()
    gfin = P.sb("gfin", [128, D], F32)
    Wgu = [P.sb("Wgu%d" % i, [128, 8, 512], BF16) for i in range(2)]
    Wd = [P.sb("Wd%d" % i, [128, 2, D], BF16) for i in range(2)]
    ot = [P.sb("ot%d" % i, [128, D], F32) for i in range(2)]

    P.dma("sp", gfin[:], g_fin_d, "c_gfin", writes=["gfin"])
    wslot = 0

    def load_expert(e):
        sl = e % 2
        wk = ("We", sl)
        P.dma("pool", Wgu[sl][:, :, 0:256], eg_w[e].rearrange("(c p) n -> p c n", p=128), "w_e%d" % sl, writes=[wk])
        P.dma("pool", Wgu[sl][:, :, 256:512], eu_w[e].rearrange("(c p) n -> p c n", p=128), "w_e%d" % sl, writes=[wk])
        P.dma("pool", Wd[sl][:], ed_w[e].rearrange("(c p) n -> p c n", p=128), "w_e%d" % sl, writes=[wk])

    load_expert(1)
    load_expert(2)
    def emit_final(lb):
            xa_ = xacc[:, lb, :]
            k_ = lb % 2
            o4 = 4 * k_
            act(P, junk[:], xa_, AF.Square, [("xacc", lb)], ["junk", ("st0", k_)], accum_out=st[:, o4:o4 + 1])
            act(P, st[:, o4 + 2:o4 + 3], st[:, o4:o4 + 1], AF.Ln, [("st0", k_)], [("st2", k_)], bias=EPS, scale=1.0 / D)
            act(P, st[:, o4 + 3:o4 + 4], st[:, o4 + 2:o4 + 3], AF.Exp, [("st2", k_)], [("st3", k_)], scale=-0.5)
            o_ = ot[lb % 2]
            stt(P, "dve", o_[:], xa_, st[:, o4 + 3:o4 + 4], gfin[:], ALU.mult, ALU.mult, [("xacc", lb), ("st3", k_), "gfin"], [("ot", lb % 2)])
            P.dma("sp", out_d[lb * 128:(lb + 1) * 128, :], o_[:], "outw%d" % (lb % 2), reads=[("ot", lb % 2)], writes=[("out", lb)])


    for e in range(16):
        sl = e % 2
        wk = ("We", sl)
        if e == 0:
            emit_expert_sub(0, 3, wgf0, wdf0, [])
        else:
            wgf = lambda c, lo, hi, w_=Wgu[sl]: w_[:, c, lo:hi]
            wdf = lambda m, n, w_=Wd[sl]: w_[:, m, n * 512:(n + 1) * 512]
            for s in range(4):
                emit_expert_sub(e, s, wgf, wdf, [wk])
                if e == 15:
                    for lb in range(4 * s, 4 * s + 4):
                        emit_final(lb)
        if e >= 1 and e + 2 < 16:
            load_expert(e + 2)
    P.phase_end()
    P.pop()

    P.outer.close()
    return nc


_NC = None
DEBUG = False


def _consts():
    idx = np.arange(128)
    ident = np.eye(128, dtype=np.float32)
    uneg = -(idx[:, None] <= idx[None, :]).astype(np.float32)
    e127 = np.zeros((128, 128), np.float32)
    e127[127, :] = 1.0
    mask01 = (idx[:, None] <= idx[None, :]).astype(np.float32)
    return ident, uneg, e127, mask01


def _bc(v, n=128):
    v = np.asarray(v, np.float32).reshape(1, -1)
    return np.ascontiguousarray(np.broadcast_to(v, (n, v.shape[1])))


def _col(v, m):
    return np.ascontiguousarray(np.asarray(v, np.float32).reshape(m, 128).T)


def kernel(x, mem, norm_mix_g, w_in, fox_bf, conv_dw_w, conv_dw_b, conv_ln_g, conv_ln_b,
           conv_pw_w, attn_branch_w, w_out, norm_xa_g, norm_mem_g, xa_wq, xa_wk, xa_wv, xa_wo,
           norm_moe_g, router_group_w, router_group_b, router_expert_w, router_expert_b,
           expert_w_gate, expert_w_up, expert_w_down, norm_final_g):
    global _NC
    if _NC is None:
        _NC = build_program()
    nc = _NC
    f = lambda a: np.ascontiguousarray(np.asarray(a, np.float32))
    x = f(x)
    mem = f(mem)
    ident, uneg, e127, mask01 = _consts()
    sel = np.zeros((128, 4, 128), np.float32)
    for hh in range(4):
        sel[2 * hh:2 * hh + 2, hh, :] = 1.0
    sel = sel.reshape(128, 512)
    dw = f(conv_dw_w)[0]
    dw_t = np.ascontiguousarray(dw.reshape(31, 4, 128).transpose(2, 1, 0)).reshape(128, 4 * 31)
    shared = {
        "w_in": f(w_in)[0], "conv_pw_w": f(conv_pw_w)[0], "attn_branch_w": f(attn_branch_w)[0],
        "w_out": f(w_out)[0], "xa_wq": f(xa_wq)[0], "xa_wk": f(xa_wk)[0], "xa_wv": f(xa_wv)[0],
        "xa_wo": f(xa_wo)[0], "expert_w_gate": f(expert_w_gate)[0], "expert_w_up": f(expert_w_up)[0],
        "expert_w_down": f(expert_w_down)[0],
        "wr": np.ascontiguousarray(np.concatenate([f(router_group_w)[0], f(router_expert_w)[0]], axis=1)),
        "rb_b": _bc(np.concatenate([f(router_group_b)[0], f(router_expert_b)[0]])),
        "g_mix_b": _bc(f(norm_mix_g)[0]), "g_xa_b": _bc(f(norm_xa_g)[0]), "g_mem_b": _bc(f(norm_mem_g)[0]),
        "g_moe_b": _bc(f(norm_moe_g)[0]), "g_fin_b": _bc(f(norm_final_g)),
        "dw_w_t": dw_t, "dw_b_t": _col(f(conv_dw_b)[0], 4), "ln_g_t": _col(f(conv_ln_g)[0], 4),
        "ln_b_t": _col(f(conv_ln_b)[0], 4),
        "ident": ident, "uneg": uneg, "e127": e127, "mask01": mask01, "sel": sel,
    }
    in_maps = []
    zeros128 = np.zeros((128, D), np.float32)
    w_in0 = shared["w_in"]
    bf0 = f(fox_bf)[0]
    for c in range(NCORES):
        b, half = c // 2, c % 2
        m = dict(shared)
        m["x_all"] = np.ascontiguousarray(x[b])
        m["x_own"] = np.ascontiguousarray(x[b, half * TOWN:(half + 1) * TOWN])
        m["x_halo"] = np.ascontiguousarray(x[b, TOWN - 128:TOWN]) if half == 1 else zeros128
        m["mem"] = np.ascontiguousarray(mem[b])
        m["flag"] = np.full((128, 1), float(half), np.float32)
        o = 256 * half
        m["w_own"] = np.ascontiguousarray(np.concatenate(
            [w_in0[:, 1024 + o:1024 + o + 256], w_in0[:, 1536 + o:1536 + o + 256],
             w_in0[:, 2048 + o:2048 + o + 256], w_in0[:, 2560 + 4 * half:2560 + 4 * half + 4]], axis=1))
        m["bf_b"] = _bc(bf0[4 * half:4 * half + 4])
        in_maps.append(m)
    if DEBUG:
        return nc, in_maps
    res = run_bass_kernel_spmd(nc, in_maps, core_ids=list(range(NCORES)))
    out = np.empty((4, 2 * TOWN, D), np.float32)
    for c in range(NCORES):
        b, half = c // 2, c % 2
        out[b, half * TOWN:(half + 1) * TOWN] = res.results[c]["out"]
    return out
```

```python
import contextlib
import numpy as np
import concourse.bass as bass
import concourse.mybir as mybir
from concourse.bass_utils import run_bass_kernel_spmd

F32 = mybir.dt.float32
BF16 = mybir.dt.bfloat16
AF = mybir.ActivationFunctionType
ALU = mybir.AluOpType

ENGS = ("pe", "act", "dve", "pool", "sp")
EPS = 1e-6
D = 1024
TOWN = 2048
NCORES = 8
EMBED_WAIT = True


class Prog:
    def __init__(self, nc):
        self.nc = nc
        self.outer = contextlib.ExitStack()
        self.stacks = [self.outer]
        self.ops = {e: [] for e in ENGS}
        self.cnt = {e: 0 for e in ENGS}
        self.esem = {e: self.outer.enter_context(nc.semaphore("s_" + e)) for e in ENGS}
        self.dsem = {}
        self.dcnt = {}
        self.last_w = {}
        self.readers = {}
        self.waited = {e: {} for e in ENGS}
        self.bank_rr = {}

    def push(self):
        self.stacks.append(contextlib.ExitStack())

    def pop(self):
        self.stacks.pop().close()

    def sb(self, name, shape, dt, outer=False):
        st = self.outer if outer else self.stacks[-1]
        return st.enter_context(self.nc.sbuf_tensor("sb_" + name, list(shape), dt))

    def ps(self, name, shape, dt):
        return self.outer.enter_context(self.nc.psum_tensor(name, list(shape), dt))

    def _dsem(self, name):
        if name not in self.dsem:
            self.dsem[name] = self.outer.enter_context(self.nc.semaphore("d_" + name))
            self.dcnt[name] = 0
        return self.dsem[name]

    def _deps(self, eng, reads, writes):
        toks = []
        for k in reads:
            t = self.last_w.get(k)
            if t is not None:
                toks.append(t)
        for k in writes:
            t = self.last_w.get(k)
            if t is not None:
                toks.append(t)
            toks.extend(self.readers.get(k, ()))
        need = {}
        for (sem, val, owner) in toks:
            if owner == eng and eng in ("pe", "sp"):
                continue
            sid = id(sem)
            if self.waited[eng].get(sid, 0) >= val:
                continue
            if sid not in need or need[sid][1] < val:
                need[sid] = (sem, val)
        for sid, (sem, val) in need.items():
            self.waited[eng][sid] = val
        return list(need.values())

    def _commit(self, tok, reads, writes):
        for k in reads:
            self.readers.setdefault(k, []).append(tok)
        for k in writes:
            self.last_w[k] = tok
            self.readers[k] = []

    def op(self, eng, fn, reads=(), writes=()):
        waits = self._deps(eng, reads, writes)
        self.cnt[eng] += 1
        val = self.cnt[eng]
        sem = self.esem[eng]

        def emit(h, fn=fn, waits=waits, sem=sem):
            if EMBED_WAIT and waits:
                for (s, v) in waits[1:]:
                    h.wait_ge(s, v)
                ins = fn(h)
                ins._wait_ge(waits[0][0], waits[0][1])
                ins.then_inc(sem, 1)
                return
            for (s, v) in waits:
                h.wait_ge(s, v)
            fn(h).then_inc(sem, 1)

        self.ops[eng].append(emit)
        self._commit((sem, val, eng), reads, writes)

    def dma(self, q, out, in_, sem_name, reads=(), writes=()):
        waits = self._deps(q, reads, writes)
        sem = self._dsem(sem_name)
        self.dcnt[sem_name] += 16
        val = self.dcnt[sem_name]

        def emit(h, waits=waits, sem=sem, out=out, in_=in_):
            for (s, v) in waits:
                h.wait_ge(s, v)
            h.dma_start(out=out, in_=in_).then_inc(sem, 16)

        self.ops[q].append(emit)
        self._commit((sem, val, "dma:" + sem_name), reads, writes)

    def collective(self, kind, ins, outs, groups, reads, writes):
        waits = self._deps("pool", reads, writes)
        sem = self._dsem("cc")
        self.dcnt["cc"] += 1
        val = self.dcnt["cc"]

        def emit(h, waits=waits, sem=sem):
            for (s, v) in waits:
                h.wait_ge(s, v)
            h.collective_compute(kind, ALU.bypass, replica_groups=groups, ins=ins, outs=outs).then_inc(sem, 1)

        self.ops["pool"].append(emit)
        self._commit((sem, val, "dma:cc"), reads, writes)

    def barrier(self):
        allw = [(self.esem[e], self.cnt[e], e) for e in ENGS if self.cnt[e] > 0]
        allw += [(self.dsem[n], self.dcnt[n], "dma:" + n) for n in self.dsem if self.dcnt[n] > 0]
        for eng in ENGS:
            waits = []
            for (sem, val, owner) in allw:
                if owner == eng:
                    continue
                if self.waited[eng].get(id(sem), 0) >= val:
                    continue
                self.waited[eng][id(sem)] = val
                waits.append((sem, val))

            def emit(h, waits=waits):
                for (s, v) in waits:
                    h.wait_ge(s, v)

            self.ops[eng].append(emit)
        self.last_w = {}
        self.readers = {}

    def phase_begin(self):
        self.push()

    def phase_end(self):
        self.barrier()
        nc = self.nc
        ops = self.ops
        with nc.Block() as block:
            @block.tensor
            def _(h):
                for f in ops["pe"]:
                    f(h)

            @block.scalar
            def _(h):
                for f in ops["act"]:
                    f(h)

            @block.vector
            def _(h):
                for f in ops["dve"]:
                    f(h)

            @block.gpsimd
            def _(h):
                for f in ops["pool"]:
                    f(h)

            @block.sync
            def _(h):
                for f in ops["sp"]:
                    f(h)
        self.ops = {e: [] for e in ENGS}
        self.pop()

    def bank(self, group, ids):
        i = self.bank_rr.get(group, 0)
        self.bank_rr[group] = i + 1
        b = ids[i % len(ids)]
        return self.banks[b], ("bank", b)


def mm(P, out, lhsT, rhs, start, stop, reads, writes):
    P.op("pe", lambda h: h.matmul(out, lhsT, rhs, start=start, stop=stop), reads, writes)


def tr(P, out, in_, ident, reads, writes):
    P.op("pe", lambda h: h.transpose(out, in_, ident), reads, writes)


def act(P, out, in_, func, reads, writes, bias=None, scale=None, accum_out=None):
    kw = {}
    if bias is not None:
        kw["bias"] = bias
    if scale is not None:
        kw["scale"] = scale
    if accum_out is not None:
        kw["accum_out"] = accum_out
    P.op("act", lambda h: h.activation(out, in_, func, **kw), reads, writes)


def tt(P, eng, out, in0, in1, op, reads, writes):
    P.op(eng, lambda h: h.tensor_tensor(out, in0, in1, op), reads, writes)


def ts(P, eng, out, in0, s1, s2, op0, op1, reads, writes):
    if s2 is None:
        P.op(eng, lambda h: h.tensor_scalar(out, in0, s1, None, op0), reads, writes)
    else:
        P.op(eng, lambda h: h.tensor_scalar(out, in0, s1, s2, op0, op1), reads, writes)


def stt(P, eng, out, in0, scalar, in1, op0, op1, reads, writes):
    P.op(eng, lambda h: h.scalar_tensor_tensor(out, in0, scalar, in1, op0, op1), reads, writes)


def cp(P, eng, out, in_, reads, writes):
    if eng == "act":
        P.op("act", lambda h: h.copy(out, in_), reads, writes)
    else:
        P.op(eng, lambda h: h.tensor_copy(out, in_), reads, writes)


def memset(P, eng, ap, val, writes):
    P.op(eng, lambda h: h.memset(ap, val), (), writes)


def build_program():
    nc = bass.Bass("TRN2", target_bir_lowering=False)

    def din(name, shape):
        return nc.dram_tensor(name, list(shape), F32, kind="ExternalInput").ap()

    x_own = din("x_own", [TOWN, D])
    x_all = din("x_all", [2 * TOWN, D])
    x_halo = din("x_halo", [128, D])
    w_own = din("w_own", [D, 772])
    mem = din("mem", [256, D])
    flag_d = din("flag", [128, 1])
    w_in = din("w_in", [D, 4616])
    pw_w = din("conv_pw_w", [512, D])
    ab_w = din("attn_branch_w", [512, D])
    wout_w = din("w_out", [D, D])
    xq_w = din("xa_wq", [D, 512])
    xk_w = din("xa_wk", [D, 512])
    xv_w = din("xa_wv", [D, 512])
    xo_w = din("xa_wo", [512, D])
    eg_w = din("expert_w_gate", [16, D, 256])
    eu_w = din("expert_w_up", [16, D, 256])
    ed_w = din("expert_w_down", [16, 256, D])
    wr_d = din("wr", [D, 20])
    rb_d = din("rb_b", [128, 20])
    g_mix_d = din("g_mix_b", [128, D])
    g_xa_d = din("g_xa_b", [128, D])
    g_mem_d = din("g_mem_b", [128, D])
    g_moe_d = din("g_moe_b", [128, D])
    g_fin_d = din("g_fin_b", [128, D])
    bf_d = din("bf_b", [128, 4])
    dww_d = din("dw_w_t", [128, 4 * 31])
    dwb_d = din("dw_b_t", [128, 4])
    lng_d = din("ln_g_t", [128, 4])
    lnb_d = din("ln_b_t", [128, 4])
    ident_d = din("ident", [128, 128])
    uneg_d = din("uneg", [128, 128])
    e127_d = din("e127", [128, 128])
    mask_d = din("mask01", [128, 128])
    out_d = nc.dram_tensor("out", [TOWN, D], F32, kind="ExternalOutput").ap()
    ag_in = nc.dram_tensor("ag_in", [256, 2 * TOWN], BF16).ap()
    ag_out = nc.dram_tensor("ag_out", [512, 2 * TOWN], BF16).ap()
    if DEBUG:
        x1_d = nc.dram_tensor("x1_scr", [TOWN, D], F32, kind="ExternalOutput").ap()
    else:
        x1_d = nc.dram_tensor("x1_scr", [TOWN, D], F32).ap()

    dbg_outs = {}

    def dump(name, ap, shape):
        if not DEBUG:
            return
        t = nc.dram_tensor("dbg_" + name, list(shape), F32, kind="ExternalOutput").ap()
        dbg_outs[name] = t
        P.dma("pool", t, ap, "dbg_" + name)

    P = Prog(nc)
    P.banks = [P.ps("bk%d" % i, [128, 512], F32) for i in range(8)]
    ALLB = list(range(8))

    attnT = P.sb("attnT", [128, 4, TOWN], BF16, outer=True)
    idb = P.sb("idb", [128, 128], BF16, outer=True)
    ones_bf = P.sb("ones_bf", [128, 128], BF16, outer=True)
    ones_f = P.sb("ones_f", [128, 128], F32, outer=True)
    xs = [P.sb("xs%d" % i, [128, D], F32, outer=True) for i in range(2)]
    junk = P.sb("junk", [128, D], BF16, outer=True)
    hb = P.sb("hb", [128, D], BF16, outer=True)
    hb2 = P.sb("hb2", [128, D], BF16, outer=True)
    hb3 = P.sb("hb3", [128, D], BF16, outer=True)
    WcG = P.sb("WcG", [128, 8, 1024], BF16, outer=True)
    KxT = P.sb("KxT", [128, 4, 256], BF16, outer=True)
    Vx = P.sb("Vx", [128, 2, 512], BF16, outer=True)
    st = P.sb("st", [128, 12], F32, outer=True)

    hbs = [hb, hb2, hb3]
    POOL_NORM = [False]
    NDEP = 2

    def emit_stats(xs_ap, xs_key, gb, gb_key, k):
        o = 4 * k
        act(P, junk[:], xs_ap, AF.Square, [xs_key], ["junk", ("st0", k)], accum_out=st[:, o:o + 1])
        act(P, st[:, o + 2:o + 3], st[:, o:o + 1], AF.Ln, [("st0", k)], [("st2", k)], bias=EPS, scale=1.0 / D)
        act(P, st[:, o + 3:o + 4], st[:, o + 2:o + 3], AF.Exp, [("st2", k)], [("st3", k)], scale=-0.5)
        stt(P, "pool" if (POOL_NORM[0] and k == 1) else "dve", hbs[k][:], xs_ap, st[:, o + 3:o + 4], gb[:],
            ALU.mult, ALU.mult, [xs_key, ("st3", k), gb_key], [("hb", k)])

    def emit_T(k, hT, hT_key, col0, evac_eng):
        bk, bkey = P.bank("any", ALLB)
        tpb = bk[:].bitcast(BF16)
        for c in range(8):
            tr(P, tpb[:, c * 128:(c + 1) * 128], hbs[k][:, c * 128:(c + 1) * 128], idb[:], [("hb", k), "idb"], [bkey])
        cp(P, evac_eng, hT[:, :, col0:col0 + 128], tpb[:, 0:1024].rearrange("p (c n) -> p c n", c=8), [bkey], [hT_key])

    ncnt = [0]

    def emit_norm(xs_ap, xs_key, gb, gb_key, hT, hT_key, col0, evac_eng):
        k = ncnt[0] % 3
        ncnt[0] += 1
        emit_stats(xs_ap, xs_key, gb, gb_key, k)
        emit_T(k, hT, hT_key, col0, evac_eng)

    def emit_norm_seq(items, gb, gb_key, pre=None):
        n = len(items)
        ks = []
        LAG = 4
        for idx in range(n + NDEP + LAG):
            if idx < n:
                it = items[idx]
                if it[6] is not None:
                    it[6]()
                k = ncnt[0] % 3
                ncnt[0] += 1
                ks.append(k)
                emit_stats(it[0], it[1], gb, gb_key, k)
            if NDEP <= idx < n + NDEP:
                it = items[idx - NDEP]
                emit_T(ks[idx - NDEP], it[2], it[3], it[4], it[5])
            if pre is not None and idx >= NDEP + LAG:
                pre(idx - NDEP - LAG)

    sel_d = din("sel", [128, 4 * 128])
    NB = 32
    NH = 4
    P.push()
    KT = P.sb("KT", [128, 2, 4096], BF16)
    QTz = P.sb("QTz", [128, NH, 4096], BF16)
    Vp = P.sb("Vp", [128, NB, 256], BF16)
    Qaug = P.sb("Qaug", [128, 4096], BF16)
    Sel = P.sb("Sel", [128, NH, 128], BF16)
    zt = P.sb("zt", [128, NB, NH], F32)
    flag = P.sb("flag", [128, 2], F32)
    maskb = P.sb("maskb", [128, 128], BF16)
    attnO = P.sb("attnO", [128, 2, 4096], BF16)

    P.phase_begin()
    Wq = P.sb("Wqkvf", [128, 8, 772], BF16)
    hT = [P.sb("hT%d" % i, [128, 8, 512], BF16) for i in range(2)]
    gmix = P.sb("gmix", [128, D], F32)
    bfb = P.sb("bfb", [128, NH], F32)

    P.dma("pool", idb[:], ident_d, "c_idb", writes=["idb"])
    P.dma("sp", gmix[:], g_mix_d, "c_gmix", writes=["gmix"])
    P.dma("sp", bfb[:], bf_d, "c_bfb", writes=["bfb"])
    P.dma("pool", Wq[:, :, 512:772], w_own[:, 512:772].rearrange("(c p) n -> p c n", p=128), "w_qv", writes=["WqV"])
    P.dma("pool", Wq[:, :, 256:512], w_own[:, 256:512].rearrange("(c p) n -> p c n", p=128), "w_qk", writes=["WqK"])
    P.dma("pool", Wq[:, :, 0:256], w_own[:, 0:256].rearrange("(c p) n -> p c n", p=128), "w_qq", writes=["WqQ"])
    P.dma("pool", maskb[:], mask_d, "c_mask", writes=["maskb"])
    P.dma("pool", Sel[:].rearrange("p a b -> p (a b)"), sel_d, "c_sel", writes=["Sel"])
    P.dma("sp", flag[:, 0:1], flag_d, "c_flag", writes=["flag"])
    memset(P, "pool", ones_bf[:], 1.0, ["ones_bf"])
    memset(P, "pool", ones_f[:], 1.0, ["ones_f"])
    memset(P, "pool", QTz[:].rearrange("p a b -> p (a b)"), 0.0, ["QTz0"])
    memset(P, "pool", Qaug[:], 0.0, ["Qaug0"])

    def a_load(blk):
        P.dma("sp", xs[blk % 2][:], x_all[blk * 128:(blk + 1) * 128, :], "xs%d" % (blk % 2), writes=[("xs", blk % 2)])

    ev = [0]

    def a_post(blk):
        ch, t = blk // 4, blk % 4
        hTc = hT[ch % 2]
        hkey = ("hT", ch % 2)
        vb, vkey = P.bank("any", ALLB)
        fb, fkey = P.bank("any", ALLB)
        for c in range(8):
            mm(P, vb[:, 0:256], hTc[:, c, t * 128:(t + 1) * 128], Wq[:, c, 512:768], c == 0, c == 7,
               [hkey, "WqV"], [vkey])
        for c in range(8):
            mm(P, fb[:, 0:NH], hTc[:, c, t * 128:(t + 1) * 128], Wq[:, c, 768:772], c == 0, c == 7,
               [hkey, "WqV"], [fkey])
        cp(P, "act", Vp[:, blk, :], vb[:, 0:256], [vkey], [("Vp", blk)])
        tt(P, "dve", zt[:, blk, :], fb[:, 0:NH], bfb[:], ALU.add, [fkey, "bfb"], [("zt", blk)])
        if t != 3:
            return
        for p in range(2):
            kb, kkey = P.bank("any", ALLB)
            for c in range(8):
                mm(P, kb[:, :], Wq[:, c, 256 + p * 128:256 + (p + 1) * 128], hTc[:, c, :], c == 0, c == 7,
                   [hkey, "WqK"], [kkey])
            cp(P, "act" if ev[0] % 2 else "dve", KT[:, p, ch * 512:(ch + 1) * 512], kb[:, :], [kkey], [("KT", p, ch)])
            ev[0] += 1
        for p in range(2):
            qb, qkey = P.bank("any", ALLB)
            for c in range(8):
                mm(P, qb[:, :], Wq[:, c, p * 128:(p + 1) * 128], hTc[:, c, :], c == 0, c == 7,
                   [hkey, "WqQ"], [qkey])
            P.op("act", lambda h, o=QTz[0:64, 2 * p, ch * 512:(ch + 1) * 512], i=qb[0:64, :]: h.mul(o, i, 0.125),
                 [qkey, "QTz0"], [("QTz", 2 * p, ch)])
            ts(P, "dve", QTz[64:128, 2 * p + 1, ch * 512:(ch + 1) * 512], qb[64:128, :], 0.125, None, ALU.mult, None,
               [qkey, "QTz0"], [("QTz", 2 * p + 1, ch)])

    items = []
    for blk in range(NB):
        ch, t = blk // 4, blk % 4
        items.append((xs[blk % 2][:], ("xs", blk % 2), hT[ch % 2], ("hT", ch % 2), t * 128,
                      "act" if blk % 2 else "dve", (lambda blk=blk: a_load(blk))))
    emit_norm_seq(items, gmix, "gmix", pre=a_post)
    P.phase_end()

    P.phase_begin()
    NZ = NB * NH
    scA = P.sb("scA", [128, NB, NH], F32)
    scB = P.sb("scB", [128, NB, NH], F32)
    excl = P.sb("excl", [128, NB, NH], F32)
    ctok = P.sb("ctok", [128, NB, NH], F32)
    cendx = P.sb("cendx", [128, NB + 1, NH], F32)
    biasT = P.sb("biasT", [128, 8, NB, NH], F32)
    lq = P.sb("lq", [128, NB, NH], F32)
    lqhi = P.sb("lqhi", [128, NB, NH], BF16)
    lqlo = P.sb("lqlo", [128, NB, NH], BF16)
    Lst = P.sb("Lst", [128, NB, NH, 2], BF16)
    uneg = P.sb("uneg", [128, 128], F32)
    oneg = P.sb("oneg", [128, 128], F32)
    e127 = P.sb("e127", [128, 128], F32)
    Pt = [P.sb("Pt%d" % i, [128, 512], BF16) for i in range(4)]
    Et = [P.sb("Et%d" % i, [128, 512], F32) for i in range(2)]
    Tm = [P.sb("Tm%d" % i, [128, 512], F32) for i in range(2)]
    rd = P.sb("rd", [128, 512], F32)
    Vh = [P.sb("Vh%d" % i, [128, NB, 128], BF16) for i in range(2)]

    P.dma("sp", uneg[:], uneg_d, "c_uneg", writes=["uneg"])
    P.dma("sp", e127[:], e127_d, "c_e127", writes=["e127"])
    P.dma("pool", WcG[:], w_in[:, 0:1024].rearrange("(c p) n -> p c n", p=128), "w_cg")
    memset(P, "pool", oneg[:], -1.0, ["oneg"])
    memset(P, "pool", excl[:, 0, :], 0.0, ["excl0"])
    memset(P, "pool", cendx[:, 0, :], 0.0, ["cendx0"])
    for b_ in range(2):
        memset(P, "pool", Vh[b_][:, :, 64:128], 1.0, [("Vh1", b_)])

    fl = lambda t_: t_[:].rearrange("p a b -> p (a b)")
    ztf = fl(zt)
    act(P, fl(scA), ztf, AF.Exp, [], ["scA"], scale=-1.0)
    act(P, ztf, fl(scA), AF.Ln, ["scA"], ["nl"], bias=1.0)
    srcT, dstT = zt, scA
    skey, dkey = "nl", "scA"
    for s_ in (1, 2, 4, 8, 16):
        tt(P, "dve", dstT[:, s_:, :], srcT[:, s_:, :], srcT[:, :NB - s_, :], ALU.add, [skey], [dkey])
        cp(P, "dve", dstT[:, :s_, :], srcT[:, :s_, :], [skey], [dkey])
        if s_ == 1:
            srcT, dstT = scA, scB
            skey, dkey = "scA", "scB"
        else:
            srcT, dstT = dstT, srcT
            skey, dkey = dkey, skey
    cp(P, "dve", excl[:, 1:, :], srcT[:, :NB - 1, :], [skey], ["excl"])
    cb, ckey = P.bank("any", ALLB)
    mm(P, cb[:, 0:NZ], uneg[:], ztf, True, False, ["uneg", "nl"], [ckey])
    mm(P, cb[:, 0:NZ], oneg[:], fl(excl), False, True, ["oneg", "excl", "excl0"], [ckey])
    cp(P, "dve", fl(ctok), cb[:, 0:NZ], [ckey], ["ctok"])
    eb, ekey = P.bank("any", ALLB)
    mm(P, eb[:, 0:NZ], e127[:], fl(ctok), True, True, ["e127", "ctok"], [ekey])
    cp(P, "dve", cendx[:, 1:, :].rearrange("p a b -> p (a b)"), eb[:, 0:NZ], [ekey, "cendx0"], ["cendx"])
    cref = cendx[:, 0:NB:4, :]
    tt(P, "dve", biasT[:], cref.unsqueeze(2).to_broadcast([128, 8, NB, NH]),
       ctok[:].unsqueeze(1).to_broadcast([128, 8, NB, NH]), ALU.subtract, ["ctok", "cendx"], ["biasT"])
    tt(P, "dve", lq[:].rearrange("p (i t) h -> p i t h", i=8), ctok[:].rearrange("p (i t) h -> p i t h", i=8),
       cref.unsqueeze(2).to_broadcast([128, 8, 4, NH]), ALU.subtract, ["ctok", "cendx"], ["lq"])
    cp(P, "dve", lqhi[:], lq[:], ["lq"], ["lqhi"])
    tt(P, "dve", lqlo[:], lq[:], lqhi[:], ALU.subtract, ["lq", "lqhi"], ["lqlo"])
    cp(P, "dve", Lst[:, :, :, 0], lqhi[:], ["lqhi"], ["Lst"])
    cp(P, "dve", Lst[:, :, :, 1], lqlo[:], ["lqlo"], ["Lst"])
    for i in range(8):
        tb, tkey = P.bank("any", ALLB)
        tpb = tb[:].bitcast(BF16)
        for t in range(4):
            tr(P, tpb[0:2 * NH, t * 128:(t + 1) * 128], Lst[:, 4 * i + t].rearrange("p a b -> p (a b)"), idb[:],
               ["Lst"], [tkey])
        cp(P, "dve", Qaug[0:2 * NH, i * 512:(i + 1) * 512], tpb[0:2 * NH, 0:512], [tkey, "Qaug0"], [("Qaug", i)])

    SB_IDS = [0, 1, 2, 3]
    O_IDS = [4, 5, 6, 7]
    NPT = 4
    SKEW = 2
    pcount = 0
    ecount = 0
    for h_ in range(NH):
        p = h_ // 2
        r0 = (h_ % 2) * 64
        vh = Vh[h_ % 2]
        vhk = ("Vh", h_ % 2)
        if h_ == 0:
            cp(P, "dve", vh[:, :, 0:64], Vp[:, :, 0:64], [], [vhk])
        for i in range(8):
            if i == 2 and h_ + 1 < NH:
                cp(P, "dve", Vh[(h_ + 1) % 2][:, :, 0:64], Vp[:, :, (h_ + 1) * 64:(h_ + 2) * 64], [],
                   [("Vh", (h_ + 1) % 2)])
            noff = 4 * i
            nj = noff + 4
            qcols = slice(i * 512, (i + 1) * 512)
            odg, odgk = P.bank("O", O_IDS)
            if noff > 0:
                ooff, ooffk = P.bank("O", O_IDS)
                ebk, ebkey = P.bank("S", SB_IDS)
                mm(P, ebk[:, :], Sel[:, h_, :], Qaug[:, qcols], True, True, [("Qaug", i)], [ebkey])
                et = Et[ecount % 2]
                etk = ("Et", ecount % 2)
                act(P, et[:], ebk[:, :], AF.Exp, [ebkey], [etk])
            ecount += 1
            pend = []

            def emit_pv(j, q0, ptile, pkey):
                if j < noff:
                    mm(P, ooff[:, :], vh[:, j, :], ptile[:, :], j == 0, j == noff - 1,
                       [vhk, ("Vh1", h_ % 2), pkey], [ooffk])
                else:
                    mm(P, odg[:, q0:512], vh[:, j, :], ptile[:, q0:512], j == noff, j == nj - 1,
                       [vhk, ("Vh1", h_ % 2), pkey], [odgk])

            for j in range(nj):
                r = j - noff
                q0 = r * 128 if r > 0 else 0
                sbk, skey_ = P.bank("S", SB_IDS)
                diag = r >= 0
                mm(P, sbk[:, q0:512], KT[:, p, j * 128:(j + 1) * 128],
                   QTz[:, h_, i * 512 + q0:(i + 1) * 512], True, not diag, [], [skey_])
                if diag:
                    mm(P, sbk[:, q0:512], Sel[:, h_, :], Qaug[:, i * 512 + q0:(i + 1) * 512], False, True,
                       [("Qaug", i)], [skey_])
                if len(pend) >= SKEW:
                    emit_pv(*pend.pop(0))
                ptile = Pt[pcount % NPT]
                pkey = ("Pt", pcount % NPT)
                pcount += 1
                act(P, ptile[:, q0:512], sbk[:, q0:512], AF.Exp, [skey_, "biasT"], [pkey],
                    bias=biasT[:, i, j, h_:h_ + 1])
                if diag:
                    tt(P, "pool", ptile[:, q0:q0 + 128], ptile[:, q0:q0 + 128], maskb[:], ALU.mult,
                       [pkey], [pkey])
                pend.append((j, q0, ptile, pkey))
            while pend:
                emit_pv(*pend.pop(0))
            tm = Tm[ecount % 2]
            tmk = ("Tm", ecount % 2)
            if noff > 0:
                tt(P, "dve", tm[:], ooff[:, :], et[:], ALU.mult, [ooffk, etk], [tmk])
                tt(P, "dve", tm[:], tm[:], odg[:, :], ALU.add, [tmk, odgk], [tmk])
            else:
                cp(P, "dve", tm[:], odg[:, :], [odgk], [tmk])
            P.op("dve", lambda h, o=rd[0:64, :], i_=tm[64:128, :]: h.reciprocal(o, i_), [tmk], ["rd"])
            tt(P, "dve", attnO[r0:r0 + 64, p, qcols], tm[0:64, :], rd[0:64, :], ALU.mult,
               [tmk, "rd"], [("attnO", p)])
        if h_ % 2 == 1:
            P.dma("sp", ag_in[p * 128:(p + 1) * 128, :], attnO[:, p, :], "ag_w%d" % p, reads=[("attnO", p)],
                  writes=[("ag_in", p)])
    P.phase_end()

    P.phase_begin()
    tA = P.sb("tA", [128, 4, TOWN], BF16)
    tB = P.sb("tB", [128, 4, TOWN], BF16)
    P.collective("AllGather", [ag_in.opt()], [ag_out.opt()], [[0, 1], [2, 3], [4, 5], [6, 7]], [], ["ag_out"])
    Wxk = P.sb("Wxk", [128, 8, 512], BF16)
    Wxv = P.sb("Wxv", [128, 8, 512], BF16)
    gmem = P.sb("gmem", [128, D], F32)
    hmT = P.sb("hmT", [128, 8, 256], BF16)
    P.dma("pool", Wxk[:], xk_w.rearrange("(c p) n -> p c n", p=128), "w_xk", writes=["Wxk"])
    P.dma("pool", Wxv[:], xv_w.rearrange("(c p) n -> p c n", p=128), "w_xv", writes=["Wxv"])
    P.dma("sp", gmem[:], g_mem_d, "c_gmem", writes=["gmem"])
    for mb in range(2):
        P.dma("sp", xs[mb][:], mem[mb * 128:(mb + 1) * 128, :], "xs%d" % mb, writes=[("xs", mb)])
        emit_norm(xs[mb][:], ("xs", mb), gmem, "gmem", hmT, "hmT", mb * 128, "dve")
    for h_ in range(4):
        kb, kkey = P.bank("any", ALLB)
        for c in range(8):
            mm(P, kb[:, 0:256], Wxk[:, c, h_ * 128:(h_ + 1) * 128], hmT[:, c, :], c == 0, c == 7, ["Wxk", "hmT"], [kkey])
        cp(P, "dve", KxT[:, h_, :], kb[:, 0:256], [kkey], ["KxT"])
    for mb in range(2):
        vb, vkey = P.bank("any", ALLB)
        for c in range(8):
            mm(P, vb[:, :], hmT[:, c, mb * 128:(mb + 1) * 128], Wxv[:, c, :], c == 0, c == 7, ["Wxv", "hmT"], [vkey])
        cp(P, "act", Vx[:, mb, :], vb[:, :], [vkey], ["Vx"])
    P.dma("sp", tA[:], ag_out[:, 0:TOWN].rearrange("(a q) n -> q a n", q=128), "ag_rA", reads=["ag_out"], writes=["tA"])
    P.dma("sp", tB[:], ag_out[:, TOWN:2 * TOWN].rearrange("(a q) n -> q a n", q=128), "ag_rB", reads=["ag_out"], writes=["tB"])
    ts(P, "dve", flag[:, 1:2], flag[:, 0:1], -1.0, 1.0, ALU.mult, ALU.add, [], ["fl2"])
    fl2 = lambda t_: t_[:].rearrange("p a b -> p (a b)")
    ts(P, "dve", fl2(tA), fl2(tA), flag[:, 1:2], None, ALU.mult, None, ["tA", "fl2"], ["tA"])
    stt(P, "dve", fl2(attnT), fl2(tB), flag[:, 0:1], fl2(tA), ALU.mult, ALU.add, ["tA", "tB"], ["attnT"])
    if DEBUG:
        P.barrier()
        dump("attnT", attnT[:].rearrange("p a b -> p (a b)"), [128, 4 * TOWN])
    P.phase_end()
    P.pop()

    P.phase_begin()
    Wc = P.sb("Wc", [128, 8, 2048], BF16)
    Wpw = P.sb("Wpw", [128, 4, D], BF16)
    Wab = P.sb("Wab", [128, 4, D], BF16)
    Wo = P.sb("Wout", [128, 8, D], BF16)
    gmix2 = P.sb("gmix2", [128, D], F32)
    dww = P.sb("dww", [128, 4, 31], F32)
    dwb = P.sb("dwb", [128, 4], F32)
    lng = P.sb("lng", [128, 4], F32)
    lnb = P.sb("lnb", [128, 4], F32)
    hTa = P.sb("hTa", [128, 8, 512], BF16)
    uT = P.sb("uT", [128, 4, 544], BF16)
    Dg = P.sb("Dg", [128, 31, 128], BF16)
    lnA = [P.sb("lnA%d" % i, [128, 512], BF16) for i in range(2)]
    lnB = [P.sb("lnB%d" % i, [128, 512], BF16) for i in range(2)]
    mgA = [P.sb("mgA%d" % i, [128, 512], BF16) for i in range(2)]
    cvb = P.sb("cvb", [128, 4, 512], BF16)
    sqb = P.sb("sqb", [128, 4, 512], BF16)
    su = P.sb("su", [128, 4, 512], BF16)
    sgc = P.sb("sgc", [128, 8, 512], BF16)
    sga = P.sb("sga", [128, 8, 512], BF16)
    mg = P.sb("mg", [128, 8, 512], BF16)
    t1 = P.sb("t1", [128, 512], F32)
    t2 = P.sb("t2", [128, 512], F32)
    t3 = P.sb("t3", [128, 512], F32)
    t4 = P.sb("t4", [128, 512], F32)
    mean_s = P.sb("mean_s", [128, 512], F32)
    rstd_s = P.sb("rstd_s", [128, 512], F32)
    oneS = P.sb("oneS", [128, 128], BF16)
    xr = [P.sb("xr%d" % i, [128, D], F32) for i in range(2)]

    P.dma("pool", Wc[:, :, 0:1024], w_in[:, 2568:3592].rearrange("(c p) n -> p c n", p=128), "w_c", writes=["Wc"])
    P.dma("pool", Wc[:, :, 1024:2048], w_in[:, 3592:4616].rearrange("(c p) n -> p c n", p=128), "w_c", writes=["Wc"])
    P.dma("pool", Wpw[:], pw_w.rearrange("(c p) n -> p c n", p=128), "w_pw", writes=["Wpw"])
    P.dma("pool", Wab[:], ab_w.rearrange("(c p) n -> p c n", p=128), "w_ab", writes=["Wab"])
    P.dma("pool", Wo[:], wout_w.rearrange("(c p) n -> p c n", p=128), "w_out", writes=["Wout"])
    P.dma("sp", gmix2[:], g_mix_d, "c_gmix2", writes=["gmix2"])
    P.dma("sp", dww[:].rearrange("p a b -> p (a b)"), dww_d, "c_dww", writes=["dww"])
    P.dma("sp", dwb[:], dwb_d, "c_dwb", writes=["dwb"])
    P.dma("sp", lng[:], lng_d, "c_lng", writes=["lng"])
    P.dma("sp", lnb[:], lnb_d, "c_lnb", writes=["lnb"])
    memset(P, "pool", oneS[:], 1.0 / 512, ["oneS"])

    def glu_tiles(ncol, dst_col0):
        for m in range(4):
            ab_, akey = P.bank("any", ALLB)
            gb_, gkey = P.bank("any", ALLB)
            for c in range(8):
                mm(P, ab_[:, 0:ncol], WcG[:, c, m * 128:(m + 1) * 128], hTa[:, c, 0:ncol], c == 0, c == 7,
                   ["hTa"], [akey])
            for c in range(8):
                mm(P, gb_[:, 0:ncol], WcG[:, c, 512 + m * 128:512 + (m + 1) * 128], hTa[:, c, 0:ncol], c == 0, c == 7,
                   ["hTa"], [gkey])
            tg = (t1, t2)[m % 2]
            tgk = ("tg", m % 2)
            act(P, tg[:, 0:ncol], gb_[:, 0:ncol], AF.Sigmoid, [gkey], [tgk])
            tt(P, "dve", uT[:, m, dst_col0:dst_col0 + ncol], ab_[:, 0:ncol], tg[:, 0:ncol], ALU.mult,
               [akey, tgk], [("uT", m)])

    P.dma("sp", xs[0][:], x_halo, "xs0", writes=[("xs", 0)])
    emit_norm(xs[0][:], ("xs", 0), gmix2, "gmix2", hTa, "hTa", 0, "dve")
    for m in range(4):
        memset(P, "pool", uT[:, m, 0:32], 0.0, [("uT", m)])
    glu_tiles(128, 32)
    for m in range(4):
        cp(P, "dve", uT[:, m, 2:32], uT[:, m, 130:160], [("uT", m)], [("uT", m)])

    def emit_gates(i, f):
        gcb, gckey = P.bank("any", ALLB)
        gab, gakey = P.bank("any", ALLB)
        for c in range(8):
            mm(P, gcb[:, :], Wc[:, c, f * 128:(f + 1) * 128], hTa[:, c, :], c == 0, c == 7,
               ["hTa", "Wc"], [gckey])
        for c in range(8):
            mm(P, gab[:, :], Wc[:, c, 1024 + f * 128:1024 + (f + 1) * 128], hTa[:, c, :], c == 0, c == 7,
               ["hTa", "Wc"], [gakey])
        act(P, sgc[:, f, :], gcb[:, :], AF.Sigmoid, [gckey], [("sgc", f)])
        act(P, sga[:, f, :], gab[:, :], AF.Sigmoid, [gakey], [("sga", f)])

    def ca_front(i):
        items = []
        for t in range(4):
            blk = i * 4 + t
            k2 = blk % 2

            def ld(blk=blk, k2=k2):
                P.dma("sp", xs[k2][:], x_own[blk * 128:(blk + 1) * 128, :], "xs%d" % k2, writes=[("xs", k2)])

            items.append((xs[k2][:], ("xs", k2), hTa, "hTa", t * 128, "act" if t % 2 else "dve", ld))
        emit_norm_seq(items, gmix2, "gmix2")

    ca_front(0)
    for i in range(4):
        def gen_dg(m):
            tt(P, "dve", Dg[:], idb[:].unsqueeze(1).to_broadcast([128, 31, 128]),
               dww[:, m, :].unsqueeze(2).to_broadcast([128, 31, 128]), ALU.mult, ["dww"], ["Dg"])

        gen_dg(0)
        glu_tiles(512, 32)
        for m in range(4):
            if m > 0:
                gen_dg(m)
            cb_, ckey_ = P.bank("any", ALLB)
            for w in range(31):
                mm(P, cb_[:, :], Dg[:, w, :], uT[:, m, 2 + w:2 + w + 512], w == 0, w == 30, ["Dg", ("uT", m)], [ckey_])
            act(P, cvb[:, m, :], cb_[:, :], AF.Identity, [ckey_, "dwb"], [("cvb", m)], bias=dwb[:, m:m + 1])
            act(P, sqb[:, m, :], cb_[:, :], AF.Square, [ckey_, "dwb"], [("sqb", m)], bias=dwb[:, m:m + 1])
            cp(P, "pool", uT[:, m, 2:32], uT[:, m, 514:544], [("uT", m)], [("uT", m)])
            emit_gates(i, 2 * m)
            emit_gates(i, 2 * m + 1)
        mb_, mkey = P.bank("any", ALLB)
        qb_, qkey_ = P.bank("any", ALLB)
        for m in range(4):
            mm(P, mb_[:, :], oneS[:], cvb[:, m, :], m == 0, m == 3, ["oneS", ("cvb", m)], [mkey])
        for m in range(4):
            mm(P, qb_[:, :], oneS[:], sqb[:, m, :], m == 0, m == 3, ["oneS", ("sqb", m)], [qkey_])
        ln_ops = []
        ln_ops.append(lambda: cp(P, "act", mean_s[:], mb_[:, :], [mkey], ["mean_s"]))
        ln_ops.append(lambda: tt(P, "dve", t2[:], mean_s[:], mean_s[:], ALU.mult, ["mean_s"], ["t2"]))
        ln_ops.append(lambda: tt(P, "dve", t3[:], qb_[:, :], t2[:], ALU.subtract, [qkey_, "t2"], ["t3"]))
        ln_ops.append(lambda: act(P, t2[:], t3[:], AF.Ln, ["t3"], ["t2"], bias=EPS))
        ln_ops.append(lambda: act(P, rstd_s[:], t2[:], AF.Exp, ["t2"], ["rstd_s"], scale=-0.5))
        ln_ops.append(lambda: stt(P, "dve", t4[:], mean_s[:], -1.0, rstd_s[:], ALU.mult, ALU.mult,
                                  ["mean_s", "rstd_s"], ["nb"]))

        def ln_m(m):
            ta = lnA[m % 2]
            tb_ = lnB[m % 2]
            tt(P, "dve", ta[:], cvb[:, m, :], rstd_s[:], ALU.mult, [("cvb", m), "rstd_s"], [("lnA", m % 2)])
            tt(P, "dve", tb_[:], ta[:], t4[:], ALU.add, [("lnA", m % 2), "nb"], [("lnB", m % 2)])
            act(P, su[:, m, :], tb_[:], AF.Silu, [("lnB", m % 2), "lng", "lnb"], [("su", m)], bias=lnb[:, m:m + 1],
                scale=lng[:, m:m + 1])

        for m in range(4):
            ln_ops.append(lambda m=m: ln_m(m))

        def ab_f(f):
            aob, aokey = P.bank("any", ALLB)
            for c in range(4):
                mm(P, aob[:, :], Wab[:, c, f * 128:(f + 1) * 128], attnT[:, c, i * 512:(i + 1) * 512], c == 0, c == 3,
                   ["Wab"], [aokey])
            tt(P, "dve", mg[:, f, :], aob[:, :], sga[:, f, :], ALU.mult, [aokey, ("sga", f)], [("mg", f)])

        ab_ops = [(lambda f=f: ab_f(f)) for f in range(8)]
        while ln_ops or ab_ops:
            if ab_ops:
                ab_ops.pop(0)()
            if ln_ops:
                ln_ops.pop(0)()
        for f in range(8):
            cob, cokey = P.bank("any", ALLB)
            for c in range(4):
                mm(P, cob[:, :], Wpw[:, c, f * 128:(f + 1) * 128], su[:, c, :], c == 0, c == 3,
                   ["Wpw", ("su", c)], [cokey])
            ma = mgA[f % 2]
            tt(P, "dve", ma[:], cob[:, :], sgc[:, f, :], ALU.mult, [cokey, ("sgc", f)], [("mgA", f % 2)])
            tt(P, "dve", mg[:, f, :], mg[:, f, :], ma[:], ALU.add, [("mgA", f % 2), ("mg", f)], [("mg", f)])
        if i + 1 < 4:
            ca_front(i + 1)
        for t in range(4):
            blk = i * 4 + t
            xb = xr[t % 2]
            xk = ("xr", t % 2)
            P.dma("sp", xb[:], x_own[blk * 128:(blk + 1) * 128, :], "xr%d" % (t % 2), writes=[xk])
            for n in range(2):
                db, dkey_ = P.bank("any", ALLB)
                for c in range(8):
                    mm(P, db[:, :], mg[:, c, t * 128:(t + 1) * 128], Wo[:, c, n * 512:(n + 1) * 512], c == 0, c == 7,
                       [("mg", c), "Wout"], [dkey_])
                tt(P, "dve", xb[:, n * 512:(n + 1) * 512], xb[:, n * 512:(n + 1) * 512], db[:, :], ALU.add,
                   [xk, dkey_], [xk])
            P.dma("sp", x1_d[blk * 128:(blk + 1) * 128, :], xb[:], "x1w%d" % (t % 2), reads=[xk], writes=[("x1", blk)])
    P.phase_end()

    P.push()
    xacc = P.sb("xacc", [128, 16, D], F32)
    h2T = P.sb("h2T", [128, 8, TOWN], BF16)
    comb = P.sb("comb", [128, 16, 16], F32)
    sa = [P.sb("sa%d" % i, [128, 512], BF16) for i in range(2)]
    uu = [P.sb("uu%d" % i, [128, 512], BF16) for i in range(4)]

    def emit_expert_ab(e, s, wgf, wkeys):
        hk = ("h2T", s)
        for m in range(2):
            ab_, akey = P.bank("any", ALLB)
            bb_, bkey = P.bank("any", ALLB)
            for c in range(8):
                mm(P, ab_[:, :], wgf(c, m * 128, (m + 1) * 128), h2T[:, c, s * 512:(s + 1) * 512], c == 0, c == 7,
                   wkeys + [hk], [akey])
            for c in range(8):
                mm(P, bb_[:, :], wgf(c, 256 + m * 128, 256 + (m + 1) * 128), h2T[:, c, s * 512:(s + 1) * 512],
                   c == 0, c == 7, wkeys + [hk], [bkey])
            act(P, sa[m][:], ab_[:, :], AF.Silu, [akey], [("sa", m)])
            tt(P, "dve", uu[(s % 2) * 2 + m][:], bb_[:, :], sa[m][:], ALU.mult, [bkey, ("sa", m)],
               [("uu", (s % 2) * 2 + m)])

    def emit_expert_y(e, s, wdf, wkeys):
        for t in range(4):
            lb = s * 4 + t
            for n in range(2):
                yb, ykey = P.bank("any", ALLB)
                for m in range(2):
                    mm(P, yb[:, :], uu[(s % 2) * 2 + m][:, t * 128:(t + 1) * 128], wdf(m, n),
                       m == 0, m == 1, [("uu", (s % 2) * 2 + m)] + wkeys, [ykey])
                stt(P, "dve", xacc[:, lb, n * 512:(n + 1) * 512], yb[:, :], comb[:, lb, e:e + 1],
                    xacc[:, lb, n * 512:(n + 1) * 512], ALU.mult, ALU.add,
                    [ykey, ("comb", lb // 4), ("xacc", lb)], [("xacc", lb)])

    def emit_expert_sub(e, s, wgf, wdf, wkeys):
        emit_expert_ab(e, s, wgf, wkeys)
        emit_expert_y(e, s, wdf, wkeys)

    wgf0 = lambda c, lo, hi: WcG[:, c, lo:hi]
    wdf0 = lambda m, n: WcG[:, 2 * m + n, 512:1024]

    P.phase_begin()
    Wxq = P.sb("Wxq", [128, 8, 512], BF16)
    Wxo = P.sb("Wxo", [128, 4, D], BF16)
    gxa = P.sb("gxa", [128, D], F32)
    h1T = P.sb("h1T", [128, 8, 512], BF16)
    qxT = P.sb("qxT", [128, 4, 512], BF16)
    Px = [P.sb("Px%d" % i, [128, 512], BF16) for i in range(4)]
    oxTs = [P.sb("oxT%d" % i, [128, 4, 512], BF16) for i in range(2)]
    rdx = P.sb("rdx", [128, 512], F32)
    P.dma("pool", Wxq[:], xq_w.rearrange("(c p) n -> p c n", p=128), "w_xq", writes=["Wxq"])
    P.dma("pool", Wxo[:], xo_w.rearrange("(c p) n -> p c n", p=128), "w_xo", writes=["Wxo"])
    P.dma("sp", gxa[:], g_xa_d, "c_gxa", writes=["gxa"])
    P.dma("pool", WcG[:, :, 0:256], eg_w[0].rearrange("(c p) n -> p c n", p=128), "w_e0p", writes=["We0"])
    P.dma("pool", WcG[:, :, 256:512], eu_w[0].rearrange("(c p) n -> p c n", p=128), "w_e0p", writes=["We0"])
    for m_ in range(2):
        P.dma("pool", WcG[:, 2 * m_:2 * m_ + 2, 512:1024],
              ed_w[0][m_ * 128:(m_ + 1) * 128, :].rearrange("p (n f) -> p n f", n=2), "w_e0p", writes=["We0"])
    Wr = P.sb("Wr", [128, 8, 20], BF16)
    rbb = P.sb("rbb", [128, 20], F32)
    gmoe = P.sb("gmoe", [128, D], F32)
    rL = P.sb("rL", [128, 4, 20], F32)
    rS = P.sb("rS", [128, 6, 4], F32)
    rG = P.sb("rG", [128, 3, 4, 4], F32)
    rE = P.sb("rE", [128, 3, 4, 16], F32)
    P.dma("pool", Wr[:], wr_d.rearrange("(c p) n -> p c n", p=128), "w_r", writes=["Wr"])
    P.dma("sp", rbb[:], rb_d, "c_rbb", writes=["rbb"])
    P.dma("sp", gmoe[:], g_moe_d, "c_gmoe", writes=["gmoe"])
    BIG = 30000.0

    def emit_moe_front(lb0):
        emit_moe_norms(lb0)
        emit_moe_router(lb0)

    def emit_moe_norms(lb0):
        for lb in range(lb0, lb0 + 4):
            emit_norm(xacc[:, lb, :], ("xacc", lb), gmoe, "gmoe", h2T, ("h2T", lb // 4), lb * 128, "act" if lb % 2 else "dve")

    def emit_moe_router(lb0):
        rb_, rkey = P.bank("any", ALLB)
        for k in range(4):
            lb = lb0 + k
            for c in range(8):
                mm(P, rb_[:, k * 20:(k + 1) * 20], h2T[:, c, lb * 128:(lb + 1) * 128], Wr[:, c, :], c == 0, c == 7,
                   [("h2T", lb // 4), "Wr"], [rkey])
        AXX = mybir.AxisListType.X
        L = rL[:]
        tt(P, "dve", L, rb_[:, 0:80].rearrange("p (k n) -> p k n", k=4), rbb[:].unsqueeze(1).to_broadcast([128, 4, 20]),
           ALU.add, [rkey, "rbb"], ["rL"])
        P.op("dve", lambda h: h.tensor_reduce(rS[:, 0, :], rL[:, :, 0:4], AXX, ALU.max), ["rL"], ["gmax"])
        tt(P, "dve", rG[:, 0], rL[:, :, 0:4], rS[:, 0, :].unsqueeze(2).to_broadcast([128, 4, 4]), ALU.is_ge,
           ["rL", "gmax"], ["gmask"])
        tt(P, "dve", rG[:, 1], rL[:, :, 0:4], rS[:, 0, :].unsqueeze(2).to_broadcast([128, 4, 4]), ALU.subtract,
           ["rL", "gmax"], ["gsh"])
        act(P, rG[:, 1], rG[:, 1], AF.Exp, ["gsh"], ["geg"])
        P.op("dve", lambda h: h.tensor_reduce(rS[:, 1, :], rG[:, 1], AXX, ALU.add), ["geg"], ["gsum"])
        ts(P, "dve", rG[:, 2], rG[:, 0], BIG, -BIG, ALU.mult, ALU.add, ["gmask"], ["pen"])
        tt(P, "dve", rE[:, 0].rearrange("p k (g e) -> p k g e", g=4),
           rL[:, :, 4:20].rearrange("p k (g e) -> p k g e", g=4),
           rG[:, 2].unsqueeze(3).to_broadcast([128, 4, 4, 4]), ALU.add, ["rL", "pen"], ["lem"])
        P.op("dve", lambda h: h.tensor_reduce(rS[:, 2, :], rE[:, 0], AXX, ALU.max), ["lem"], ["m1"])
        tt(P, "dve", rE[:, 1], rE[:, 0], rS[:, 2, :].unsqueeze(2).to_broadcast([128, 4, 16]), ALU.is_ge,
           ["lem", "m1"], ["mask1"])
        stt(P, "dve", rE[:, 2], rE[:, 1], -BIG, rE[:, 0], ALU.mult, ALU.add, ["mask1", "lem"], ["lem2"])
        P.op("dve", lambda h: h.tensor_reduce(rS[:, 3, :], rE[:, 2], AXX, ALU.max), ["lem2"], ["m2"])
        tt(P, "dve", rE[:, 1], rE[:, 0], rS[:, 3, :].unsqueeze(2).to_broadcast([128, 4, 16]), ALU.is_ge,
           ["lem", "m2", "lem2"], ["selm"])
        tt(P, "dve", rE[:, 2], rE[:, 0], rS[:, 2, :].unsqueeze(2).to_broadcast([128, 4, 16]), ALU.subtract,
           ["lem", "m1", "selm"], ["esh"])
        act(P, rE[:, 2], rE[:, 2], AF.Exp, ["esh"], ["eex"])
        tt(P, "dve", rE[:, 2], rE[:, 2], rE[:, 1], ALU.mult, ["eex", "selm"], ["eex2"])
        P.op("dve", lambda h: h.tensor_reduce(rS[:, 4, :], rE[:, 2], AXX, ALU.add), ["eex2"], ["ssum"])
        tt(P, "dve", rS[:, 5, :], rS[:, 1, :], rS[:, 4, :], ALU.mult, ["gsum", "ssum"], ["coef0"])
        P.op("dve", lambda h: h.reciprocal(rS[:, 5, :], rS[:, 5, :]), ["coef0"], ["coef"])
        tt(P, "dve", comb[:, lb0:lb0 + 4, :], rE[:, 2], rS[:, 5, :].unsqueeze(2).to_broadcast([128, 4, 16]), ALU.mult,
           ["eex2", "coef"], [("comb", lb0 // 4)])


    XS = float(128 ** -0.5)
    pxc = [0]

    def cb_A(s):
        oxT = oxTs[s % 2]
        items = []
        for t in range(4):
            lb = s * 4 + t

            def ld(lb=lb):
                P.dma("sp", xacc[:, lb, :], x1_d[lb * 128:(lb + 1) * 128, :], "xacc%d" % lb, writes=[("xacc", lb)])

            items.append((xacc[:, lb, :], ("xacc", lb), h1T, "h1T", t * 128, "act" if t % 2 else "dve", ld))
        emit_norm_seq(items, gxa, "gxa")
        for h_ in range(4):
            qb, qkey = P.bank("any", ALLB)
            for c in range(8):
                mm(P, qb[:, :], Wxq[:, c, h_ * 128:(h_ + 1) * 128], h1T[:, c, :], c == 0, c == 7, ["Wxq", "h1T"], [qkey])
            P.op("act", lambda h, o=qxT[:, h_, :], i_=qb[:, :]: h.mul(o, i_, XS), [qkey], [("qxT", h_)])
        def x_s1(h_):
            pts = []
            for mb in range(2):
                sb_, skey_ = P.bank("any", ALLB)
                mm(P, sb_[:, :], KxT[:, h_, mb * 128:(mb + 1) * 128], qxT[:, h_, :], True, True,
                   [("qxT", h_)], [skey_])
                ptile = Px[pxc[0] % 4]
                pkey = ("Px", pxc[0] % 4)
                pxc[0] += 1
                act(P, ptile[:], sb_[:, :], AF.Exp, [skey_], [pkey])
                pts.append((ptile, pkey))
            return pts

        def x_s2(h_, pts):
            db, dkey_ = P.bank("any", ALLB)
            ob, okey = P.bank("any", ALLB)
            for mb in range(2):
                mm(P, db[:, :], ones_bf[:], pts[mb][0][:], mb == 0, mb == 1, [pts[mb][1]], [dkey_])
            for mb in range(2):
                mm(P, ob[:, :], Vx[:, mb, h_ * 128:(h_ + 1) * 128], pts[mb][0][:], mb == 0, mb == 1,
                   [pts[mb][1]], [okey])
            act(P, rdx[:], db[:, :], AF.Ln, [dkey_], ["rdx"])
            act(P, rdx[:], rdx[:], AF.Exp, ["rdx"], ["rdx"], scale=-1.0)
            tt(P, "dve", oxT[:, h_, :], ob[:, :], rdx[:], ALU.mult, [okey, "rdx"], [("oxT", s % 2, h_)])

        prev = None
        for h_ in range(4):
            cur = (h_, x_s1(h_))
            if prev is not None:
                x_s2(*prev)
            prev = cur
        x_s2(*prev)

    def cb_B(s, fill=None):
        oxT = oxTs[s % 2]
        for t in range(4):
            lb = s * 4 + t
            for n in range(2):
                wb_, wkey = P.bank("any", ALLB)
                for c in range(4):
                    mm(P, wb_[:, :], oxT[:, c, t * 128:(t + 1) * 128], Wxo[:, c, n * 512:(n + 1) * 512], c == 0, c == 3,
                       [("oxT", s % 2, c), "Wxo"], [wkey])
                tt(P, "dve", xacc[:, lb, n * 512:(n + 1) * 512], xacc[:, lb, n * 512:(n + 1) * 512], wb_[:, :], ALU.add,
                   [("xacc", lb), wkey], [("xacc", lb)])
        if fill is None:
            emit_moe_front(s * 4)
        else:
            fill(0)
            emit_moe_norms(s * 4)
            fill(1)
            emit_moe_router(s * 4)
            fill(2)

    cb_A(0)
    for s in range(4):
        if s + 1 < 4:
            cb_A(s + 1)
        if s == 3:
            cb_B(s, fill=lambda s0: emit_expert_sub(0, s0, wgf0, wdf0, ["We0"]))
        else:
            cb_B(s)
    POOL_NORM[0] = False
    P.phase_end()

    P.phase_begin()
    gfin = P.sb("gfin", [128, D], F32)
    Wgu = [P.sb("Wgu%d" % i, [128, 8, 512], BF16) for i in range(2)]
    Wd = [P.sb("Wd%d" % i, [128, 2, D], BF16) for i in range(2)]
    ot = [P.sb("ot%d" % i, [128, D], F32) for i in range(2)]

    P.dma("sp", gfin[:], g_fin_d, "c_gfin", writes=["gfin"])
    wslot = 0

    def load_expert(e):
        sl = e % 2
        wk = ("We", sl)
        P.dma("pool", Wgu[sl][:, :, 0:256], eg_w[e].rearrange("(c p) n -> p c n", p=128), "w_e%d" % sl, writes=[wk])
        P.dma("pool", Wgu[sl][:, :, 256:512], eu_w[e].rearrange("(c p) n -> p c n", p=128), "w_e%d" % sl, writes=[wk])
        P.dma("pool", Wd[sl][:], ed_w[e].rearrange("(c p) n -> p c n", p=128), "w_e%d" % sl, writes=[wk])

    load_expert(1)
    load_expert(2)
    def emit_final(lb):
            xa_ = xacc[:, lb, :]
            k_ = lb % 2
            o4 = 4 * k_
            act(P, junk[:], xa_, AF.Square, [("xacc", lb)], ["junk", ("st0", k_)], accum_out=st[:, o4:o4 + 1])
            act(P, st[:, o4 + 2:o4 + 3], st[:, o4:o4 + 1], AF.Ln, [("st0", k_)], [("st2", k_)], bias=EPS, scale=1.0 / D)
            act(P, st[:, o4 + 3:o4 + 4], st[:, o4 + 2:o4 + 3], AF.Exp, [("st2", k_)], [("st3", k_)], scale=-0.5)
            o_ = ot[lb % 2]
            stt(P, "dve", o_[:], xa_, st[:, o4 + 3:o4 + 4], gfin[:], ALU.mult, ALU.mult, [("xacc", lb), ("st3", k_), "gfin"], [("ot", lb % 2)])
            P.dma("sp", out_d[lb * 128:(lb + 1) * 128, :], o_[:], "outw%d" % (lb % 2), reads=[("ot", lb % 2)], writes=[("out", lb)])


    emit_expert_sub(0, 3, wgf0, wdf0, [])
    items = []
    for e in range(1, 16):
        sl = e % 2
        wgf = (lambda c, lo, hi, w_=Wgu[sl]: w_[:, c, lo:hi])
        wdf = (lambda m, n, w_=Wd[sl]: w_[:, m, n * 512:(n + 1) * 512])
        for s_ in range(4):
            items.append((e, s_, wgf, wdf, [("We", sl)]))
    emit_expert_ab(items[0][0], items[0][1], items[0][2], items[0][4])
    for k, (e, s_, wgf, wdf, wks) in enumerate(items):
        if k + 1 < len(items):
            e2, s2, wgf2, wdf2, wks2 = items[k + 1]
            emit_expert_ab(e2, s2, wgf2, wks2)
        emit_expert_y(e, s_, wdf, wks)
        if e == 15:
            for lb in range(4 * s_, 4 * s_ + 4):
                emit_final(lb)
        if s_ == 3 and e + 2 < 16:
            load_expert(e + 2)
    P.phase_end()
    P.pop()

    P.outer.close()
    return nc


_NC = None
DEBUG = False


def _consts():
    idx = np.arange(128)
    ident = np.eye(128, dtype=np.float32)
    uneg = -(idx[:, None] <= idx[None, :]).astype(np.float32)
    e127 = np.zeros((128, 128), np.float32)
    e127[127, :] = 1.0
    mask01 = (idx[:, None] <= idx[None, :]).astype(np.float32)
    return ident, uneg, e127, mask01


def _bc(v, n=128):
    v = np.asarray(v, np.float32).reshape(1, -1)
    return np.ascontiguousarray(np.broadcast_to(v, (n, v.shape[1])))


def _col(v, m):
    return np.ascontiguousarray(np.asarray(v, np.float32).reshape(m, 128).T)


def kernel(x, mem, norm_mix_g, w_in, fox_bf, conv_dw_w, conv_dw_b, conv_ln_g, conv_ln_b,
           conv_pw_w, attn_branch_w, w_out, norm_xa_g, norm_mem_g, xa_wq, xa_wk, xa_wv, xa_wo,
           norm_moe_g, router_group_w, router_group_b, router_expert_w, router_expert_b,
           expert_w_gate, expert_w_up, expert_w_down, norm_final_g):
    global _NC
    if _NC is None:
        _NC = build_program()
    nc = _NC
    f = lambda a: np.ascontiguousarray(np.asarray(a, np.float32))
    x = f(x)
    mem = f(mem)
    ident, uneg, e127, mask01 = _consts()
    sel = np.zeros((128, 4, 128), np.float32)
    for hh in range(4):
        sel[2 * hh:2 * hh + 2, hh, :] = 1.0
    sel = sel.reshape(128, 512)
    dw = f(conv_dw_w)[0]
    dw_t = np.ascontiguousarray(dw.reshape(31, 4, 128).transpose(2, 1, 0)).reshape(128, 4 * 31)
    shared = {
        "w_in": f(w_in)[0], "conv_pw_w": f(conv_pw_w)[0], "attn_branch_w": f(attn_branch_w)[0],
        "w_out": f(w_out)[0], "xa_wq": f(xa_wq)[0], "xa_wk": f(xa_wk)[0], "xa_wv": f(xa_wv)[0],
        "xa_wo": f(xa_wo)[0], "expert_w_gate": f(expert_w_gate)[0], "expert_w_up": f(expert_w_up)[0],
        "expert_w_down": f(expert_w_down)[0],
        "wr": np.ascontiguousarray(np.concatenate([f(router_group_w)[0], f(router_expert_w)[0]], axis=1)),
        "rb_b": _bc(np.concatenate([f(router_group_b)[0], f(router_expert_b)[0]])),
        "g_mix_b": _bc(f(norm_mix_g)[0]), "g_xa_b": _bc(f(norm_xa_g)[0]), "g_mem_b": _bc(f(norm_mem_g)[0]),
        "g_moe_b": _bc(f(norm_moe_g)[0]), "g_fin_b": _bc(f(norm_final_g)),
        "dw_w_t": dw_t, "dw_b_t": _col(f(conv_dw_b)[0], 4), "ln_g_t": _col(f(conv_ln_g)[0], 4),
        "ln_b_t": _col(f(conv_ln_b)[0], 4),
        "ident": ident, "uneg": uneg, "e127": e127, "mask01": mask01, "sel": sel,
    }
    in_maps = []
    zeros128 = np.zeros((128, D), np.float32)
    w_in0 = shared["w_in"]
    bf0 = f(fox_bf)[0]
    for c in range(NCORES):
        b, half = c // 2, c % 2
        m = dict(shared)
        m["x_all"] = np.ascontiguousarray(x[b])
        m["x_own"] = np.ascontiguousarray(x[b, half * TOWN:(half + 1) * TOWN])
        m["x_halo"] = np.ascontiguousarray(x[b, TOWN - 128:TOWN]) if half == 1 else zeros128
        m["mem"] = np.ascontiguousarray(mem[b])
        m["flag"] = np.full((128, 1), float(half), np.float32)
        o = 256 * half
        m["w_own"] = np.ascontiguousarray(np.concatenate(
            [w_in0[:, 1024 + o:1024 + o + 256], w_in0[:, 1536 + o:1536 + o + 256],
             w_in0[:, 2048 + o:2048 + o + 256], w_in0[:, 2560 + 4 * half:2560 + 4 * half + 4]], axis=1))
        m["bf_b"] = _bc(bf0[4 * half:4 * half + 4])
        in_maps.append(m)
    if DEBUG:
        return nc, in_maps
    res = run_bass_kernel_spmd(nc, in_maps, core_ids=list(range(NCORES)))
    out = np.empty((4, 2 * TOWN, D), np.float32)
    for c in range(NCORES):
        b, half = c // 2, c % 2
        out[b, half * TOWN:(half + 1) * TOWN] = res.results[c]["out"]
    return out
```
